# Optimizing a Trainium2 kernel written in Bass

```python
import jax, jax.numpy as jnp
from jax import lax
import numpy as np

D_MODEL = 1024
BATCH = 16
SEQ = 2048
DEPTH = 2

N_EVEN = (DEPTH + 1) // 2
N_ODD = DEPTH // 2

DN_HEADS = 4
DN_DK = 128
DN_DV = 128
DN_CHUNK = 64
CONV_WIDTH = 4
FOX_HEADS = 4
FOX_DH = 128
FOX_BLOCK = 128
EVEN_MIX = DN_HEADS * DN_DV + FOX_HEADS * FOX_DH
EVEN_SIZES = (2 * DN_HEADS * DN_DK + DN_HEADS * DN_DV,
              DN_HEADS * DN_DV,
              DN_HEADS,
              DN_HEADS,
              FOX_HEADS * FOX_DH, FOX_HEADS * FOX_DH, FOX_HEADS * FOX_DH,
              FOX_HEADS)
EVEN_IN = sum(EVEN_SIZES)

LRU_WIDTH = 512
LRU_BLOCKS = 4
LRU_C = 8.0
SGU_WIDTH = 512
SGU_GROUPS = 4
SGU_CHUNK = 128
ODD_MIX = LRU_WIDTH + SGU_WIDTH
ODD_SIZES = (LRU_WIDTH, LRU_WIDTH, SGU_WIDTH, SGU_WIDTH)
ODD_IN = sum(ODD_SIZES)

N_EXPERTS = 16
N_EXPERT_GROUPS = 4
EXPERTS_PER_GROUP = N_EXPERTS // N_EXPERT_GROUPS
TOP_K = 2
D_EXPERT = D_MODEL // 4

EPS = 1e-6

kernel_name = "hybrid_deltanet_fox_rglru_sgu_moe"


def split_cols(t, sizes):
    offs = np.cumsum(np.array(sizes))[:-1].tolist()
    return jnp.split(t, offs, axis=-1)


def rmsnorm(x, g):
    xf = x.astype(jnp.float32)
    y = xf * lax.rsqrt(jnp.mean(xf * xf, axis=-1, keepdims=True) + EPS)
    return (y * g.astype(jnp.float32)).astype(x.dtype)


def l2norm(x):
    xf = x.astype(jnp.float32)
    return xf * lax.rsqrt(jnp.sum(xf * xf, axis=-1, keepdims=True) + EPS)


def modulate(h, shift, scale):
    return h * (1 + scale[:, None, :]) + shift[:, None, :]


def causal_dwconv(x, w):
    k_w, ch = w.shape
    return lax.conv_general_dilated(x, w[:, None, :].astype(x.dtype), window_strides=(1,),
                                    padding=[(k_w - 1, 0)],
                                    dimension_numbers=('NWC', 'WIO', 'NWC'),
                                    feature_group_count=ch)


def gated_deltanet(q, k, v, log_decay, beta):
    f32 = jnp.float32
    b_, s_, h_, dk = q.shape
    dv = v.shape[-1]
    c_ = DN_CHUNK
    n_ = s_ // c_
    q = l2norm(q) * (dk ** -0.5)
    k = l2norm(k)
    v = v.astype(f32)

    def chunks(t):
        t = t.reshape((b_, n_, c_, h_) + t.shape[3:])
        return jnp.moveaxis(t, (1, 3), (0, 2))

    qc, kc, vc = chunks(q), chunks(k), chunks(v)
    gc = jnp.cumsum(chunks(log_decay.astype(f32)), axis=-1)
    bc = chunks(beta.astype(f32))
    idx = jnp.arange(c_)
    causal = idx[:, None] >= idx[None, :]
    strict = idx[:, None] > idx[None, :]
    decay = jnp.exp(jnp.where(causal, gc[..., :, None] - gc[..., None, :], -jnp.inf))
    kk = jnp.einsum('nbhcd,nbhsd->nbhcs', kc * bc[..., None], kc) * decay
    eye = jnp.eye(c_, dtype=f32)
    lower = eye + jnp.where(strict, kk, 0.0)
    t_inv = lax.linalg.triangular_solve(lower, jnp.broadcast_to(eye, lower.shape),
                                        left_side=True, lower=True)
    u = jnp.matmul(t_inv, vc * bc[..., None])
    w = jnp.matmul(t_inv, kc * (bc * jnp.exp(gc))[..., None])
    qk = jnp.einsum('nbhcd,nbhsd->nbhcs', qc, kc) * decay
    q_dec = qc * jnp.exp(gc)[..., None]
    k_dec = kc * jnp.exp(gc[..., -1:] - gc)[..., None]
    g_last = jnp.exp(gc[..., -1])

    def step(state, inp):
        u_i, w_i, qk_i, qd_i, kd_i, gl_i = inp
        v_new = u_i - jnp.matmul(w_i, state)
        o_i = jnp.matmul(qd_i, state) + jnp.matmul(qk_i, v_new)
        state = state * gl_i[..., None, None] + jnp.matmul(jnp.swapaxes(kd_i, -1, -2), v_new)
        return state, o_i

    s0 = jnp.zeros((b_, h_, dk, dv), f32)
    _, o = lax.scan(step, s0, (u, w, qk, q_dec, k_dec, g_last))
    return jnp.moveaxis(o, (0, 2), (1, 3)).reshape(b_, s_, h_, dv)


def forgetting_attention(q, k, v, log_f):
    f32 = jnp.float32
    s_ = q.shape[2]
    scale = q.shape[-1] ** -0.5
    cum = jnp.cumsum(log_f, axis=-1)
    outs = []
    for blk in range(s_ // FOX_BLOCK):
        q0 = blk * FOX_BLOCK
        q1 = q0 + FOX_BLOCK
        kb, vb = k[:, :, :q1], v[:, :, :q1]
        logits = (jnp.einsum('bhqd,bhkd->bhqk', q[:, :, q0:q1], kb).astype(f32) * scale
                  + cum[:, :, q0:q1, None] - cum[:, :, None, :q1])
        mask = (q0 + jnp.arange(FOX_BLOCK))[:, None] >= jnp.arange(q1)[None, :]
        p = jax.nn.softmax(jnp.where(mask, logits, -jnp.inf), axis=-1)
        outs.append(jnp.einsum('bhqk,bhkd->bhqd', p.astype(vb.dtype), vb))
    return jnp.concatenate(outs, axis=2)


def rg_lru(x, w_r, b_r, w_i, b_i, lam):
    f32 = jnp.float32
    b_, s_, wd = x.shape
    xh = x.reshape(b_, s_, LRU_BLOCKS, wd // LRU_BLOCKS)
    r = jax.nn.sigmoid(jnp.einsum('bshi,hij->bshj', xh, w_r).reshape(b_, s_, wd) + b_r)
    i = jax.nn.sigmoid(jnp.einsum('bshi,hij->bshj', xh, w_i).reshape(b_, s_, wd) + b_i)
    log_a = -LRU_C * r.astype(f32) * jax.nn.softplus(-lam.astype(f32))
    a = jnp.exp(log_a)
    b = jnp.sqrt(-jnp.expm1(2.0 * log_a)) * (i * x).astype(f32)

    def combine(left, right):
        a1, b1 = left
        a2, b2 = right
        return a1 * a2, a2 * b1 + b2

    _, h = lax.associative_scan(combine, (a, b), axis=1)
    return h.astype(x.dtype)


def chunked_sgu(u, v, g_norm, w_s, b_s):
    b_, s_, wd = u.shape
    n_ = s_ // SGU_CHUNK
    v = rmsnorm(v, g_norm).reshape(b_, n_, SGU_CHUNK, SGU_GROUPS, wd // SGU_GROUPS)
    tri = jnp.tril(jnp.ones((SGU_CHUNK, SGU_CHUNK), dtype=bool))
    w = jnp.where(tri, w_s, 0.0).astype(v.dtype)
    mixed = jnp.einsum('gts,bnsgd->bntgd', w, v) + b_s.T[None, None, :, :, None]
    return u * mixed.reshape(b_, s_, wd)


def even_mixer(h, w_in, conv_w, a_log, dt_bias, onorm_g, f_bias, qnorm_g, knorm_g, w_out):
    f32 = jnp.float32
    b_, s_, _ = h.shape
    proj = h @ w_in
    dn_qkv, dn_z, dn_a, dn_b, fq, fk, fv, ff = split_cols(proj, EVEN_SIZES)
    dn_qkv = jax.nn.silu(causal_dwconv(dn_qkv, conv_w))
    dq, dk, dv = split_cols(dn_qkv, (DN_HEADS * DN_DK, DN_HEADS * DN_DK, DN_HEADS * DN_DV))
    dq = dq.reshape(b_, s_, DN_HEADS, DN_DK)
    dk = dk.reshape(b_, s_, DN_HEADS, DN_DK)
    dv = dv.reshape(b_, s_, DN_HEADS, DN_DV)
    log_decay = -jnp.exp(a_log.astype(f32)) * jax.nn.softplus(dn_a.astype(f32) + dt_bias.astype(f32))
    beta = jax.nn.sigmoid(dn_b.astype(f32))
    o_dn = gated_deltanet(dq, dk, dv, log_decay, beta)
    o_dn = rmsnorm(o_dn, onorm_g) * jax.nn.silu(dn_z.reshape(b_, s_, DN_HEADS, DN_DV).astype(f32))
    o_dn = o_dn.reshape(b_, s_, DN_HEADS * DN_DV).astype(h.dtype)
    fq = rmsnorm(fq.reshape(b_, s_, FOX_HEADS, FOX_DH), qnorm_g).transpose(0, 2, 1, 3)
    fk = rmsnorm(fk.reshape(b_, s_, FOX_HEADS, FOX_DH), knorm_g).transpose(0, 2, 1, 3)
    fv = fv.reshape(b_, s_, FOX_HEADS, FOX_DH).transpose(0, 2, 1, 3)
    log_f = jax.nn.log_sigmoid(ff.astype(f32) + f_bias.astype(f32)).transpose(0, 2, 1)
    o_fox = forgetting_attention(fq, fk, fv, log_f)
    o_fox = o_fox.transpose(0, 2, 1, 3).reshape(b_, s_, FOX_HEADS * FOX_DH).astype(h.dtype)
    return jnp.concatenate([o_dn, o_fox], axis=-1) @ w_out


def odd_mixer(h, w_in, conv_w, conv_b, wr, br, wi, bi, lam, sgu_g, sgu_w, sgu_b, w_out):
    proj = h @ w_in
    lx, lg, su, sv = split_cols(proj, ODD_SIZES)
    lx = causal_dwconv(lx, conv_w) + conv_b
    o_lru = rg_lru(lx, wr, br, wi, bi, lam) * jax.nn.gelu(lg)
    o_sgu = chunked_sgu(jax.nn.gelu(su), jax.nn.gelu(sv), sgu_g, sgu_w, sgu_b)
    return jnp.concatenate([o_lru, o_sgu], axis=-1) @ w_out


def moe(h, router_w, router_b, w_gate, w_up, w_down):
    f32 = jnp.float32
    b_, s_, _ = h.shape
    probs = jax.nn.softmax(jnp.einsum('bsd,de->bse', h, router_w).astype(f32), axis=-1)
    sel = (probs + router_b.astype(f32)).reshape(b_, s_, N_EXPERT_GROUPS, EXPERTS_PER_GROUP)
    group_score = jnp.sum(lax.top_k(sel, TOP_K)[0], axis=-1)
    g_idx = jnp.argmax(group_score, axis=-1)
    in_group = jnp.sum(sel * jax.nn.one_hot(g_idx, N_EXPERT_GROUPS, dtype=f32)[..., None], axis=2)
    _, local = lax.top_k(in_group, TOP_K)
    e_idx = g_idx[..., None] * EXPERTS_PER_GROUP + local
    wts = jnp.take_along_axis(probs, e_idx, axis=-1)
    wts = wts / jnp.sum(wts, axis=-1, keepdims=True)
    gates = jnp.sum(jax.nn.one_hot(e_idx, N_EXPERTS, dtype=f32) * wts[..., None], axis=-2)
    act = (jax.nn.silu(jnp.einsum('bsd,edf->bsef', h, w_gate))
           * jnp.einsum('bsd,edf->bsef', h, w_up)) * gates.astype(h.dtype)[..., None]
    return jnp.einsum('bsef,efd->bsd', act, w_down)


def setup_inputs(seed: int = 0) -> dict:
    key = jax.random.key(seed)
    ks = list(jax.random.split(key, 40))
    f32 = jnp.float32
    d = D_MODEL

    def nrm(shape, scale):
        return jax.random.normal(ks.pop(), shape, f32) * scale

    def gain(shape):
        return 1.0 + nrm(shape, 0.02)

    x = nrm((BATCH, SEQ, d), 1.0)
    c = nrm((BATCH, d), 1.0)
    ada_w = nrm((DEPTH, d, 6 * d), 0.5 * d ** -0.5)
    ada_b = nrm((DEPTH, 6 * d), 0.02)
    norm1_g = gain((DEPTH, d))
    norm2_g = gain((DEPTH, d))
    ev_w_in = nrm((N_EVEN, d, EVEN_IN), d ** -0.5)
    ev_conv_w = nrm((N_EVEN, CONV_WIDTH, EVEN_SIZES[0]), CONV_WIDTH ** -0.5)
    ev_dn_a_log = jnp.log(jax.random.uniform(ks.pop(), (N_EVEN, DN_HEADS), f32, 1.0, 16.0))
    dt = jnp.exp(jax.random.uniform(ks.pop(), (N_EVEN, DN_HEADS), f32,
                                    float(np.log(1e-3)), float(np.log(1e-1))))
    ev_dn_dt_bias = dt + jnp.log(-jnp.expm1(-dt))
    ev_dn_onorm_g = gain((N_EVEN, DN_DV))
    ev_fox_f_bias = 2.0 + nrm((N_EVEN, FOX_HEADS), 0.5)
    ev_fox_qnorm_g = gain((N_EVEN, FOX_DH))
    ev_fox_knorm_g = gain((N_EVEN, FOX_DH))
    ev_w_out = nrm((N_EVEN, EVEN_MIX, d), EVEN_MIX ** -0.5)
    blk = LRU_WIDTH // LRU_BLOCKS
    od_w_in = nrm((N_ODD, d, ODD_IN), d ** -0.5)
    od_conv_w = nrm((N_ODD, CONV_WIDTH, LRU_WIDTH), CONV_WIDTH ** -0.5)
    od_conv_b = nrm((N_ODD, LRU_WIDTH), 0.02)
    od_lru_wr = nrm((N_ODD, LRU_BLOCKS, blk, blk), blk ** -0.5)
    od_lru_br = nrm((N_ODD, LRU_WIDTH), 0.02)
    od_lru_wi = nrm((N_ODD, LRU_BLOCKS, blk, blk), blk ** -0.5)
    od_lru_bi = nrm((N_ODD, LRU_WIDTH), 0.02)
    a0 = jax.random.uniform(ks.pop(), (N_ODD, LRU_WIDTH), f32, 0.9, 0.999) ** (1.0 / LRU_C)
    od_lru_lambda = jnp.log(a0) - jnp.log1p(-a0)
    od_sgu_norm_g = gain((N_ODD, SGU_WIDTH))
    od_sgu_w = nrm((N_ODD, SGU_GROUPS, SGU_CHUNK, SGU_CHUNK), SGU_CHUNK ** -0.5)
    od_sgu_b = 1.0 + nrm((N_ODD, SGU_GROUPS, SGU_CHUNK), 0.02)
    od_w_out = nrm((N_ODD, ODD_MIX, d), ODD_MIX ** -0.5)
    router_w = nrm((d, N_EXPERTS), d ** -0.5)
    router_b = nrm((N_EXPERTS,), 0.01)
    moe_w_gate = nrm((DEPTH, N_EXPERTS, d, D_EXPERT), d ** -0.5)
    moe_w_up = nrm((DEPTH, N_EXPERTS, d, D_EXPERT), d ** -0.5)
    moe_w_down = nrm((DEPTH, N_EXPERTS, D_EXPERT, d), D_EXPERT ** -0.5)
    return {"x": x, "c": c, "ada_w": ada_w, "ada_b": ada_b, "norm1_g": norm1_g, "norm2_g": norm2_g,
            "ev_w_in": ev_w_in, "ev_conv_w": ev_conv_w, "ev_dn_a_log": ev_dn_a_log,
            "ev_dn_dt_bias": ev_dn_dt_bias, "ev_dn_onorm_g": ev_dn_onorm_g, "ev_fox_f_bias": ev_fox_f_bias,
            "ev_fox_qnorm_g": ev_fox_qnorm_g, "ev_fox_knorm_g": ev_fox_knorm_g, "ev_w_out": ev_w_out,
            "od_w_in": od_w_in, "od_conv_w": od_conv_w, "od_conv_b": od_conv_b, "od_lru_wr": od_lru_wr,
            "od_lru_br": od_lru_br, "od_lru_wi": od_lru_wi, "od_lru_bi": od_lru_bi,
            "od_lru_lambda": od_lru_lambda, "od_sgu_norm_g": od_sgu_norm_g, "od_sgu_w": od_sgu_w,
            "od_sgu_b": od_sgu_b, "od_w_out": od_w_out, "router_w": router_w, "router_b": router_b,
            "moe_w_gate": moe_w_gate, "moe_w_up": moe_w_up, "moe_w_down": moe_w_down}


def reference(x, c, ada_w, ada_b, norm1_g, norm2_g,
              ev_w_in, ev_conv_w, ev_dn_a_log, ev_dn_dt_bias, ev_dn_onorm_g, ev_fox_f_bias,
              ev_fox_qnorm_g, ev_fox_knorm_g, ev_w_out,
              od_w_in, od_conv_w, od_conv_b, od_lru_wr, od_lru_br, od_lru_wi, od_lru_bi,
              od_lru_lambda, od_sgu_norm_g, od_sgu_w, od_sgu_b, od_w_out,
              router_w, router_b, moe_w_gate, moe_w_up, moe_w_down):
    c_act = jax.nn.silu(c)
    for layer in range(DEPTH):
        mod = c_act @ ada_w[layer] + ada_b[layer]
        sh1, sc1, gt1, sh2, sc2, gt2 = jnp.split(mod, 6, axis=-1)
        h = modulate(rmsnorm(x, norm1_g[layer]), sh1, sc1)
        i = layer // 2
        if layer % 2 == 0:
            y = even_mixer(h, ev_w_in[i], ev_conv_w[i], ev_dn_a_log[i], ev_dn_dt_bias[i],
                           ev_dn_onorm_g[i], ev_fox_f_bias[i], ev_fox_qnorm_g[i], ev_fox_knorm_g[i],
                           ev_w_out[i])
        else:
            y = odd_mixer(h, od_w_in[i], od_conv_w[i], od_conv_b[i], od_lru_wr[i], od_lru_br[i],
                          od_lru_wi[i], od_lru_bi[i], od_lru_lambda[i], od_sgu_norm_g[i],
                          od_sgu_w[i], od_sgu_b[i], od_w_out[i])
        x = x + gt1[:, None, :] * y
        h = modulate(rmsnorm(x, norm2_g[layer]), sh2, sc2)
        x = x + gt2[:, None, :] * moe(h, router_w, router_b, moe_w_gate[layer], moe_w_up[layer],
                                      moe_w_down[layer])
    return x
```

```python
import numpy as np
from contextlib import ExitStack
import concourse.bass as bass
import concourse.mybir as mybir
from concourse.bass_utils import run_bass_kernel_spmd

F32 = mybir.dt.float32
BF16 = mybir.dt.bfloat16
AF = mybir.ActivationFunctionType
ALU = mybir.AluOpType
AX = mybir.AxisListType


class Prog:
    PHYS = ['pe', 'act', 'dve', 'pool', 'sp']
    QUEUES = ['pe', 'act', 'dve', 'pool', 'sp', 'poolq', 'actq']
    ISSUER = {'pe': 'pe', 'act': 'act', 'dve': 'dve', 'pool': 'pool', 'sp': 'sp', 'poolq': 'pool', 'actq': 'act'}
    DMAQ = ('sp', 'poolq', 'actq')

    NSLOT = 8

    def __init__(self, nc):
        self.nc = nc
        self.es = ExitStack()
        self.root = self.es
        self.streams = {e: [] for e in self.PHYS}
        self.nsem = 0
        self.sems = {}
        self.count = {}
        self.ndma = {q: 0 for q in self.DMAQ}
        self.waited = {}
        self.lastw = {}
        self.readers = {}
        self._new_sems()
        self.n_ops = 0
        self.n_waits = 0

    def _semkeys(self):
        ks = []
        for q in self.QUEUES:
            if q in self.DMAQ:
                ks += [(q, i) for i in range(self.NSLOT)]
            else:
                ks.append(q)
        return ks

    def _new_sems(self):
        for sk in self._semkeys():
            nm = sk if isinstance(sk, str) else "%s%d" % sk
            self.sems[sk] = self.root.enter_context(self.nc.semaphore("s%s_%d" % (nm, self.nsem)))
            self.count[sk] = 0
        self.nsem += 1
        self.waited = {e: {sk: 0 for sk in self._semkeys()} for e in self.PHYS}
        self.lastw = {}
        self.readers = {}

    def sb(self, name, shape, dtype):
        self.uid = getattr(self, 'uid', 0) + 1
        return self.es.enter_context(self.nc.sbuf_tensor("%s_u%d" % (name, self.uid), list(shape), dtype))

    def ps(self, name, shape, dtype=F32):
        self.uid = getattr(self, 'uid', 0) + 1
        return self.es.enter_context(self.nc.psum_tensor("%s_u%d" % (name, self.uid), list(shape), dtype))

    def _inc(self, sk):
        return 1 if isinstance(sk, str) else 16

    def _need(self, phys, sk, idx):
        if self.waited[phys][sk] >= idx:
            return
        self.waited[phys][sk] = idx
        self.streams[phys].append(('wait', self.sems[sk], idx * self._inc(sk)))
        self.n_waits += 1

    def op(self, q, fn, reads=(), writes=()):
        phys = self.ISSUER[q]
        deps = set()
        for k in reads:
            if k in self.lastw:
                deps.add(self.lastw[k])
        for k in writes:
            if k in self.lastw:
                deps.add(self.lastw[k])
            for r in self.readers.get(k, ()):
                deps.add(r)
        for (dsk, didx) in deps:
            if dsk == 'pe' and q == 'pe':
                continue
            self._need(phys, dsk, didx)
        if q in self.DMAQ:
            sk = (q, self.ndma[q] % self.NSLOT)
            self.ndma[q] += 1
            self._need(phys, sk, self.count[sk])
        else:
            sk = q
        self.count[sk] += 1
        idx = self.count[sk]
        self.streams[phys].append(('op', fn, self.sems[sk], self._inc(sk)))
        me = (sk, idx)
        for k in reads:
            self.readers.setdefault(k, []).append(me)
        for k in writes:
            self.lastw[k] = me
            self.readers[k] = []
        self.n_ops += 1
        return me

    def dma(self, q, out, in_, reads=(), writes=(), **kw):
        return self.op(q, lambda e: e.dma_start(out=out, in_=in_, **kw), reads, writes)

    def barrier(self):
        for phys in self.PHYS:
            for sk in self._semkeys():
                if self.count[sk] > 0:
                    self._need(phys, sk, self.count[sk])
        self.lastw = {}
        self.readers = {}
        if max(self.count[sk] * self._inc(sk) for sk in self._semkeys()) > 24000:
            self._new_sems()

    def finish(self, out_keys=()):
        for k in out_keys:
            if k in self.lastw:
                self._need('sp', *self.lastw[k])
        self.barrier_final()
        nc = self.nc
        streams = self.streams

        def emit(eng, items):
            for it in items:
                if it[0] == 'wait':
                    eng.wait_ge(it[1], it[2])
                else:
                    ins = it[1](eng)
                    ins.then_inc(it[2], it[3])

        with nc.Block() as block:
            @block.tensor
            def _(e):
                emit(e, streams['pe'])

            @block.scalar
            def _(e):
                emit(e, streams['act'])

            @block.vector
            def _(e):
                emit(e, streams['dve'])

            @block.gpsimd
            def _(e):
                emit(e, streams['pool'])

            @block.sync
            def _(e):
                emit(e, streams['sp'])
        self.es.close()

    def barrier_final(self):
        for sk in self._semkeys():
            if self.count[sk] > 0:
                self._need('sp', sk, self.count[sk])

    def push(self):
        if not hasattr(self, '_stack'):
            self._stack = []
        self._stack.append(self.es)
        self.es = ExitStack()

    def pop(self):
        self.barrier()
        self.es.close()
        self.es = self._stack.pop()

    def mm(self, out, lhsT, rhs, start=True, stop=True, reads=(), writes=()):
        return self.op('pe', lambda e: e.matmul(out, lhsT, rhs, start=start, stop=stop), reads, writes)

    def tr(self, out, in_, ident, reads=(), writes=()):
        return self.op('pe', lambda e: e.transpose(out, in_, ident), reads, writes)

    def act(self, out, in_, func, reads=(), writes=(), **kw):
        return self.op('act', lambda e: e.activation(out, in_, func, **kw), reads, writes)

    def tt(self, q, out, a, b, op, reads=(), writes=()):
        return self.op(q, lambda e: e.tensor_tensor(out, a, b, op), reads, writes)

    def ts(self, q, out, in0, s1, s2, op0, op1=None, reads=(), writes=(), **kw):
        if op1 is None:
            return self.op(q, lambda e: e.tensor_scalar(out, in0, s1, None, op0, **kw), reads, writes)
        return self.op(q, lambda e: e.tensor_scalar(out, in0, s1, s2, op0, op1, **kw), reads, writes)

    def stt(self, out, in0, scalar, in1, op0, op1, reads=(), writes=()):
        return self.op('dve', lambda e: e.scalar_tensor_tensor(out, in0, scalar, in1, op0, op1), reads, writes)

    def cp(self, q, out, in_, reads=(), writes=()):
        if q == 'act':
            return self.op('act', lambda e: e.copy(out, in_), reads, writes)
        return self.op(q, lambda e: e.tensor_copy(out, in_), reads, writes)

    def recip(self, out, in_, reads=(), writes=()):
        return self.op('dve', lambda e: e.reciprocal(out, in_), reads, writes)

    def memset(self, q, ap, val, writes=()):
        return self.op(q, lambda e: e.memset(ap, val), (), writes)


T = 4096
S = 2048
D = 1024
NT = 32
EPS = 1e-6
OFF = {'sh1': 0, 'sc1': 1024, 'gt1': 2048, 'sh2': 3072, 'sc2': 4096, 'gt2': 5120}


def kl(name, n):
    return [(name, i) for i in range(n)]


class K:
    pass


def setup_consts(P, k):
    nc = P.nc
    k.id32 = P.sb("id32", [128, 128], F32)
    k.idb = P.sb("idb", [128, 128], BF16)
    k.ones32 = P.sb("ones32", [128, 128], F32)
    k.onesb = P.sb("onesb", [128, 128], BF16)
    k.mU = P.sb("mU", [128, 2, 128], F32)
    k.ltri = P.sb("ltri", [128, 2, 128], F32)
    k.mUf = P.sb("mUf", [128, 128], BF16)
    P.memset('pool', k.ones32[:], 1.0, ['ones32'])
    P.memset('pool', k.onesb[:], 1.0, ['onesb'])
    P.op('pool', lambda e: e.affine_select(k.id32[:], k.ones32[:], [[-1, 128]], ALU.is_equal, 0.0, base=0, channel_multiplier=1),
         ['ones32'], ['id32'])
    P.cp('pool', k.idb[:], k.id32[:], ['id32'], ['idb'])
    tmp = P.sb("ctmp", [128, 128], F32)
    P.op('pool', lambda e: e.affine_select(tmp[:], k.ones32[:], [[1, 128]], ALU.is_ge, 0.0, base=0, channel_multiplier=-1),
         ['ones32'], ['ctmp'])
    P.cp('pool', k.mUf[:], tmp[:], ['ctmp'], ['mUf'])
    P.cp('pool', k.mU[:, 0, :], tmp[:], ['ctmp'], ['mU'])
    P.memset('pool', k.mU[0:64, 0, 64:128], 0.0, ['mU'])
    P.tt('pool', k.mU[:, 1, :], k.mU[:, 0, :], k.id32[:], ALU.subtract, ['mU', 'id32'], ['mU'])
    P.cp('pool', k.ltri[:, 0, :], k.mU[:, 0, :], ['mU'], ['ltri'])
    P.memset('pool', k.ltri[:, 1, :], 0.0, ['ltri'])
    P.memset('pool', k.ltri[0:64, 1, 0:64], 1.0, ['ltri'])
    P.memset('pool', k.ltri[64:128, 1, 64:128], 1.0, ['ltri'])


def phase_mod(P, k):
    P.push()
    cT = P.sb("cT", [128, 8, 2], F32)
    scT = P.sb("scT", [128, 8, 2], F32)
    ab = P.sb("ab", [2, 2, 6144], F32)
    msb = P.sb("msb", [2, 2, 6144], F32)
    wa = [P.sb("wa%d" % i, [128, 8, 512], F32) for i in range(2)]
    psm = [P.ps("psm%d" % i, [128, 512]) for i in range(2)]
    for b in range(2):
        P.dma('sp', cT[:, :, b], k.c[b, :].rearrange("(kc p) -> p kc", p=128), [], ['cT'], allow_slow_non_contiguous=True)
    P.act(scT[:], cT[:], AF.Silu, ['cT'], ['scT'])
    for l in range(2):
        P.dma('sp', ab[:, l, :], k.ada_b[l:l + 1, :].partition_broadcast(2), [], ['ab'])
    i = 0
    for l in range(2):
        for cb in range(12):
            j = i % 2
            P.dma('sp' if j == 0 else 'actq', wa[j][:], k.ada_w[l, :, cb * 512:(cb + 1) * 512].rearrange("(kc p) n -> p kc n", p=128),
                  [], ['wa%d' % j])
            for kc in range(8):
                P.mm(psm[j][0:2, :], scT[:, kc, :], wa[j][:, kc, :], kc == 0, kc == 7, ['scT', 'wa%d' % j], ['psm%d' % j])
            P.tt('dve', msb[:, l, cb * 512:(cb + 1) * 512], psm[j][0:2, :], ab[:, l, cb * 512:(cb + 1) * 512], ALU.add,
                 ['psm%d' % j, 'ab'], ['msb'])
            i += 1
    P.dma('sp', k.modrows.rearrange("l b n -> b l n"), msb[:], ['msb'], ['modrows'])
    P.pop()


class Normer:
    def __init__(self, P, k, l, which, src, f32):
        self.P, self.k, self.src, self.f32 = P, k, src, f32
        g_ap = (k.norm1_g if which == 1 else k.norm2_g)
        gcol = P.sb("n_gcol", [128, 8], F32)
        P.dma('sp', gcol[:], g_ap[l, :].rearrange("(kc p) -> p kc", p=128), [], ['n_gcol'], allow_slow_non_contiguous=True)
        self.A, self.B = [], []
        for b in range(2):
            sc = P.sb("n_sc%d" % b, [128, 8], F32)
            A = P.sb("n_A%d" % b, [128, 8], F32)
            B = P.sb("n_B%d" % b, [128, 8], F32)
            o = OFF['sc%d' % which]
            P.dma('sp', sc[:], k.modrows[l, b, o:o + 1024].rearrange("(kc p) -> p kc", p=128), [], ['n_sc%d' % b],
                  allow_slow_non_contiguous=True)
            o = OFF['sh%d' % which]
            P.dma('sp', B[:], k.modrows[l, b, o:o + 1024].rearrange("(kc p) -> p kc", p=128), [], ['n_B%d' % b],
                  allow_slow_non_contiguous=True)
            P.stt(A[:], sc[:], 1.0, gcol[:], ALU.add, ALU.mult, ['n_sc%d' % b, 'n_gcol'], ['n_A%d' % b])
            self.A.append(A)
            self.B.append(B)
        dt = F32 if f32 else BF16
        self.xt = [P.sb("n_xt%d" % i, [128, 1024], F32) for i in range(2)]
        self.xn = [P.sb("n_xn%d" % i, [128, 1024], dt) for i in range(2)]
        self.junk = P.sb("n_junk", [128, 1024], BF16)
        self.st = P.sb("n_st", [128, 2, 4], F32)
        self.pst = [P.ps("n_pst%d" % i, [128, 8, 128], dt) for i in range(2)]
        self.ident = k.id32 if f32 else k.idb
        self.i = 0

    def tile(self, ti, out_fn, out_keys):
        self.tile_a(ti)
        self.tile_b(ti, out_fn, out_keys)

    def tile_a(self, ti):
        P, k = self.P, self.k
        j = self.i % 2
        self.i += 1
        self.slot = getattr(self, 'slot', {})
        self.slot[ti] = j
        xt, xn, st = self.xt[j], self.xn[j], self.st
        kx, kn, ks = 'n_xt%d' % j, 'n_xn%d' % j, 'n_st%d' % j
        P.dma('sp', xt[:], self.src[ti * 128:(ti + 1) * 128, :], ['xsrc%d' % ti], [kx])
        P.act(self.junk[:], xt[:], AF.Square, [kx], ['n_junk', ks], accum_out=st[:, j, 0:1])
        P.act(st[:, j, 1:2], st[:, j, 0:1], AF.Sqrt, [ks], [ks], bias=EPS, scale=1.0 / 1024)
        P.recip(st[:, j, 2:3], st[:, j, 1:2], [ks], [ks])
        if self.f32:
            P.act(xn[:], xt[:], AF.Copy, [kx, ks], [kn], scale=st[:, j, 2:3])
        else:
            P.ts('dve', xn[:], xt[:], st[:, j, 2:3], None, ALU.mult, None, [kx, ks], [kn])

    def tile_b(self, ti, out_fn, out_keys):
        P, k = self.P, self.k
        j = self.slot[ti]
        b = ti // 16
        xn, pst = self.xn[j], self.pst[j]
        kn, kp = 'n_xn%d' % j, 'n_pst%d' % j
        for kc in range(8):
            P.tr(pst[:, kc, :], xn[:, kc * 128:(kc + 1) * 128], self.ident[:], [kn, 'idb', 'id32'], [kp])
        for kc in range(8):
            dst = out_fn(kc)
            if j == 0:
                P.act(dst, pst[:, kc, :], AF.Identity, [kp, 'n_A%d' % b, 'n_B%d' % b], [out_keys[kc]],
                      bias=self.B[b][:, kc:kc + 1], scale=self.A[b][:, kc:kc + 1])
            else:
                P.ts('dve', dst, pst[:, kc, :], self.A[b][:, kc:kc + 1], self.B[b][:, kc:kc + 1], ALU.mult, ALU.add,
                     [kp, 'n_A%d' % b, 'n_B%d' % b], [out_keys[kc]])


def load_w_bf16(P, dst, src, key, nsplit=8):
    kcs = dst.shape[1]
    for kc in range(kcs):
        P.dma('poolq', dst[:, kc, :], src[kc * 128:(kc + 1) * 128, :], [], [key])


def ftile_col(f):
    return f * 128 if f < 16 else 2056 + (f - 16) * 128


def phase_e1(P, k, xsrc):
    P.push()
    W = P.sb("e1_W", [128, 8, 3596], BF16)
    load_w_bf16(P, W, k.ev_w_in[0], 'e1_W')
    nm = Normer(P, k, 0, 1, xsrc, False)
    hT = [P.sb("e1_hT%d" % i, [128, 8, 512], BF16) for i in range(2)]
    psf = [P.ps("e1_psf%d" % i, [128, 512]) for i in range(3)]
    psv = P.ps("e1_psv", [128, 512])
    psab = P.ps("e1_psab", [128, 4, 8])
    psff = P.ps("e1_psff", [128, 512])
    stg = [P.sb("e1_stg%d" % i, [128, 512], BF16) for i in range(4)]
    stv = [P.sb("e1_stv%d" % i, [128, 512], BF16) for i in range(2)]
    stab = [P.sb("e1_stab%d" % i, [128, 4, 8], F32) for i in range(2)]
    ff = P.sb("e1_ff", [4, T], F32)
    fbc = P.sb("e1_fbc", [4, 2], F32)
    P.dma('sp', fbc[:, 0:1], k.ev_fox_f_bias.rearrange("o h -> h o"), [], ['e1_fbc'], allow_slow_non_contiguous=True)
    P.ts('dve', fbc[:, 1:2], fbc[:, 0:1], -1.0, None, ALU.mult, None, ['e1_fbc'], ['e1_fbc'])
    nst = [0]

    def norm_a(tb, j):
        nm.tile_a(tb * 4 + j)

    def norm_b(tb, j):
        h = hT[tb % 2]
        hk = [('e1_hT%d' % (tb % 2), kc) for kc in range(8)]
        nm.tile_b(tb * 4 + j, lambda kc, h=h, j=j: h[:, kc, j * 128:(j + 1) * 128], hk)

    def norm_tile(tb, j):
        norm_a(tb, j)
        norm_b(tb, j)

    def fillers(tb):
        fs = [lambda: norm_a(tb, 0)]
        for j in range(1, 4):
            fs.append(lambda j=j: (norm_b(tb, j - 1), norm_a(tb, j)))
        fs.append(lambda: norm_b(tb, 3))
        return fs

    def units(tb):
        h = hT[tb % 2]
        hk = [('e1_hT%d' % (tb % 2), kc) for kc in range(8)]
        us = []

        def ftile(f):
            ib = f % 3
            ps = psf[ib]
            c0 = ftile_col(f)
            for kc in range(8):
                P.mm(ps[:], W[:, kc, c0:c0 + 128], h[:, kc, :], kc == 0, kc == 7, ['e1_W'] + hk, ['e1_psf%d' % ib])
            sg = stg[nst[0] % 4]
            sk = 'e1_stg%d' % (nst[0] % 4)
            nst[0] += 1
            P.cp('act' if ib != 1 else 'dve', sg[:], ps[:], ['e1_psf%d' % ib], [sk])
            P.dma('sp', k.projT[f * 128:(f + 1) * 128, tb * 512:(tb + 1) * 512], sg[:], [sk], [('projT', f, tb)])

        def fvtile(j):
            for kc in range(8):
                P.mm(psv[:], h[:, kc, j * 128:(j + 1) * 128], W[:, kc, 3080:3592], kc == 0, kc == 7, ['e1_W'] + hk, ['e1_psv'])
            sv = stv[j % 2]
            P.cp('act', sv[:], psv[:], ['e1_psv'], ['e1_stv%d' % (j % 2)])
            t0 = tb * 512 + j * 128
            P.dma('sp', k.fvtm[t0:t0 + 128, :], sv[:], ['e1_stv%d' % (j % 2)], [('fvtm', tb, j)])
            for kc in range(8):
                P.mm(psab[:, j, :], h[:, kc, j * 128:(j + 1) * 128], W[:, kc, 2048:2056], kc == 0, kc == 7, ['e1_W'] + hk, ['e1_psab'])

        def tail():
            sa = stab[tb % 2]
            P.cp('dve', sa[:], psab[:], ['e1_psab'], ['e1_stab%d' % (tb % 2)])
            P.dma('sp', k.abtm[tb * 512:(tb + 1) * 512, :].rearrange("(j p) c -> p j c", p=128), sa[:], ['e1_stab%d' % (tb % 2)],
                  [('abtm', tb)])
            for kc in range(8):
                P.mm(psff[0:4, :], W[:, kc, 3592:3596], h[:, kc, :], kc == 0, kc == 7, ['e1_W'] + hk, ['e1_psff'])
            P.cp('dve', ff[:, tb * 512:(tb + 1) * 512], psff[0:4, :], ['e1_psff'], ['e1_ff'])

        for f in range(24):
            us.append(lambda f=f: ftile(f))
        for j in range(4):
            us.append(lambda j=j: fvtile(j))
        us.append(tail)
        return us

    for j in range(4):
        norm_tile(0, j)
    for tb in range(8):
        us = units(tb)
        fill = fillers(tb + 1) if tb + 1 < 8 else []
        for i, u_ in enumerate(us):
            u_()
            if fill and i % 6 == 4:
                fill.pop(0)()
        while fill:
            fill.pop(0)()
    P.dma('sp', k.ffrows, ff[:], ['e1_ff'], ['ffrows'])
    P.pop()


def phase_e1b(P, k):
    P.push()
    ff = P.sb("e1b_ff", [4, T], F32)
    fbc = P.sb("e1b_fbc", [4, 2], F32)
    P.dma('sp', ff[:], k.ffrows, [], ['e1b_ff'])
    P.dma('sp', fbc[:, 0:1], k.ev_fox_f_bias.rearrange("o h -> h o"), [], ['e1b_fbc'], allow_slow_non_contiguous=True)
    P.ts('dve', fbc[:, 1:2], fbc[:, 0:1], -1.0, None, ALU.mult, None, ['e1b_fbc'], ['e1b_fbc'])
    lf = P.sb("e1b_lf", [4, T], F32)
    cum = P.sb("e1b_cum", [4, 2, T], F32)
    one4 = P.sb("e1b_one4", [4, S], F32)
    P.memset('pool', one4[:], 1.0, ['e1b_one4'])
    P.act(lf[:], ff[:], AF.Exp, ['e1b_ff', 'e1b_fbc'], ['e1b_lf'], bias=fbc[:, 1:2], scale=-1.0)
    P.act(lf[:], lf[:], AF.Ln, ['e1b_lf'], ['e1b_lf'], bias=1.0, scale=1.0)
    P.ts('dve', lf[:], lf[:], -1.0, None, ALU.mult, None, ['e1b_lf'], ['e1b_lf'])
    for s in range(2):
        P.op('dve', lambda e, s=s: e.tensor_tensor_scan(cum[:, 0, s * S:(s + 1) * S], one4[:], lf[:, s * S:(s + 1) * S], 0.0,
                                                         ALU.mult, ALU.add), ['e1b_lf', 'e1b_one4'], ['e1b_cum'])
    P.ts('dve', cum[:, 1, :], cum[:, 0, :], -1.0, None, ALU.mult, None, ['e1b_cum'], ['e1b_cum'])
    P.dma('sp', k.cumrows.rearrange("(a h) t -> h a t", a=2), cum[:], ['e1b_cum'], ['cumrows'])
    hl = P.sb("e1b_hl", [4, 4, T], BF16)
    hif = P.sb("e1b_hif", [4, T], F32)
    for a in range(2):
        kd = 2 * (1 - a)
        P.cp('act', hl[:, kd, :], cum[:, a, :], ['e1b_cum'], ['e1b_hl'])
        P.cp('act', hif[:], hl[:, kd, :], ['e1b_hl'], ['e1b_hif'])
        P.tt('dve', hl[:, kd + 1, :], cum[:, a, :], hif[:], ALU.subtract, ['e1b_cum', 'e1b_hif'], ['e1b_hl'])
    P.dma('sp', k.cumhl, hl[:], ['e1b_hl'], ['cumhl'])
    P.pop()


IN_SPECS = [("x", [T, D]), ("c", [2, D]), ("ada_w", [2, 1024, 6144]), ("ada_b", [2, 6144]), ("norm1_g", [2, 1024]),
            ("norm2_g", [2, 1024]), ("ev_w_in", [1, 1024, 3596]), ("ev_conv_w", [1, 4, 1536]), ("ev_dn_a_log", [1, 4]),
            ("ev_dn_dt_bias", [1, 4]), ("ev_dn_onorm_g", [1, 128]), ("ev_fox_f_bias", [1, 4]), ("ev_fox_qnorm_g", [1, 128]),
            ("ev_fox_knorm_g", [1, 128]), ("ev_w_out", [1, 1024, 1024]), ("od_w_in", [1, 1024, 2048]),
            ("od_conv_w", [1, 4, 512]), ("od_conv_b", [1, 512]), ("od_lru_wr", [1, 4, 128, 128]), ("od_lru_br", [1, 512]),
            ("od_lru_wi", [1, 4, 128, 128]), ("od_lru_bi", [1, 512]), ("od_lru_lambda", [1, 512]),
            ("od_sgu_norm_g", [1, 512]), ("od_sgu_w", [1, 4, 128, 128]), ("od_sgu_b", [1, 4, 128]),
            ("od_w_out", [1, 1024, 1024]), ("router_w", [1024, 16]), ("router_b", [16]),
            ("moe_w_gate", [2, 16, 1024, 256]), ("moe_w_up", [2, 16, 1024, 256]), ("moe_w_down", [2, 16, 256, 1024])]

SCRATCH = [("modrows", [2, 2, 6144], F32), ("projT", [3072, T], BF16), ("fvtm", [T, 512], BF16), ("abtm", [T, 8], F32),
           ("cumrows", [8, T], F32), ("ffrows", [4, T], F32), ("cumhl", [4, 4, T], BF16), ("dnT", [1536, T], BF16), ("omixT", [1024, T], BF16),
           ("xa", [T, D], F32), ("xb", [T, D], F32), ("proj1T", [1536, T], BF16), ("svtm", [T, 512], BF16)]


def build(dbg=(), stop=None):
    nc = bass.Bass("TRN2", target_bir_lowering=False)
    k = K()
    for name, shape in IN_SPECS:
        setattr(k, name, nc.dram_tensor(name, list(shape), F32, kind="ExternalInput").ap())
    for name, shape, dt in SCRATCH:
        kind = "ExternalOutput" if name in dbg else "Internal"
        setattr(k, name, nc.dram_tensor(name, list(shape), dt, kind=kind).ap())
    k.out = nc.dram_tensor("out", [T, D], F32, kind="ExternalOutput").ap()
    if "dbg_gates" in dbg:
        k.dbg_gates = nc.dram_tensor("dbg_gates", [2, 128, 16, 16], F32, kind="ExternalOutput").ap()
    P = Prog(nc)
    setup_consts(P, k)
    phases = [
        ('mod', lambda: phase_mod(P, k)),
        ('e1', lambda: (phase_e1(P, k, k.x), phase_e1b(P, k))),
        ('e3', lambda: phase_e3(P, k)),
        ('e2a', lambda: phase_e2a(P, k)),
        ('e2b', lambda: phase_e2b(P, k)),
        ('e4', lambda: phase_outproj(P, k, k.ev_w_out[0], 0, k.x, k.xa, 'e4')),
        ('m0', lambda: phase_moe(P, k, 0, k.xa, k.xb, 'm0')),
        ('o1', lambda: phase_o1(P, k, k.xb)),
        ('o2', lambda: phase_o2(P, k)),
        ('o3', lambda: phase_o3(P, k)),
        ('o4', lambda: phase_outproj(P, k, k.od_w_out[0], 1, k.xb, k.xa, 'o4')),
        ('m1', lambda: phase_moe(P, k, 1, k.xa, k.out, 'm1')),
    ]
    for name, fn in phases:
        fn()
        if stop == name:
            break
    P.finish()
    return nc, P


def make_in_map(inputs, core):
    m = {}
    for name, shape in IN_SPECS:
        a = inputs[name]
        if name == "x":
            a = a[2 * core:2 * core + 2].reshape(T, D)
        elif name == "c":
            a = a[2 * core:2 * core + 2]
        m[name] = np.ascontiguousarray(a, dtype=np.float32)
    return m


def phase_e3(P, k):
    P.push()
    gq = P.sb("e3_gq", [128, 2], F32)
    gs = P.sb("e3_gs", [128, 2], F32)
    negm = P.sb("e3_negm", [128, 1], F32)
    mrow = P.sb("e3_mrow", [1, 4], F32)
    psx = P.ps("e3_psx", [128, 512])
    P.dma('sp', gq[:, 0:1], k.ev_fox_qnorm_g.rearrange("o d -> d o"), [], ['e3_gq'], allow_slow_non_contiguous=True)
    P.dma('sp', gq[:, 1:2], k.ev_fox_knorm_g.rearrange("o d -> d o"), [], ['e3_gq'], allow_slow_non_contiguous=True)
    P.ts('dve', gs[:, 0:1], gq[:, 0:1], 128 ** -0.5, None, ALU.mult, None, ['e3_gq'], ['e3_gs'])
    P.cp('dve', gs[:, 1:2], gq[:, 1:2], ['e3_gq'], ['e3_gs'])
    for i in range(2):
        P.mm(psx[0:1, i * 128:(i + 1) * 128], gq[:, i:i + 1], k.id32[:], True, True, ['e3_gq', 'id32'], ['e3_psx'])
        P.op('dve', lambda e, i=i: e.tensor_reduce(mrow[:, i:i + 1], psx[0:1, i * 128:(i + 1) * 128], AX.X, ALU.max,
                                                   apply_absolute_value=True), ['e3_psx'], ['e3_mrow'])
    P.tt('dve', mrow[:, 2:3], mrow[:, 0:1], mrow[:, 1:2], ALU.mult, ['e3_mrow'], ['e3_mrow'])
    P.mm(psx[:, 256:257], k.ones32[0:1, :], mrow[:, 2:3], True, True, ['e3_mrow', 'ones32'], ['e3_psx'])
    P.ts('dve', negm[:], psx[:, 256:257], -(128 ** 0.5) * 1.02, None, ALU.mult, None, ['e3_psx'], ['e3_negm'])

    raw = [[P.sb("e3_raw%d_%d" % (p_, i), [128, S], BF16) for i in range(2)] for p_ in range(2)]
    sq = [P.sb("e3_sq%d" % i, [128, 512], BF16) for i in range(2)]
    rt = [P.sb("e3_rt%d" % i, [128, 512], F32) for i in range(2)]
    qn = [P.sb("e3_qn%d" % i, [128, 512], F32) for i in range(2)]
    qk = [[P.sb("e3_qk%d_%d" % (p_, i), [128, S], BF16) for i in range(2)] for p_ in range(2)]
    V = [P.sb("e3_V%d" % p_, [128, 16, 128], BF16) for p_ in range(2)]
    CK = [P.sb("e3_CK%d" % p_, [4, S], BF16) for p_ in range(2)]
    CQ = [P.sb("e3_CQ%d" % p_, [4, S], BF16) for p_ in range(2)]
    for p_ in range(2):
        P.memset('pool', CK[p_][:], 1.0, ['e3_CK%d' % p_])
        P.memset('pool', CQ[p_][:], 1.0, ['e3_CQ%d' % p_])
    psn = P.ps("e3_psn", [128, 512])
    pss = [P.ps("e3_pss%d" % i, [128, 512]) for i in range(2)]
    pso = [P.ps("e3_pso%d" % i, [128, 512]) for i in range(2)]
    psd = [P.ps("e3_psd%d" % i, [128, 512]) for i in range(2)]
    ptb = [P.sb("e3_pt%d" % i, [128, 512], BF16) for i in range(3)]
    rden = P.sb("e3_rden", [128, 512], F32)
    ot = [P.sb("e3_ot%d" % i, [128, 512], BF16) for i in range(2)]
    heads = [(s_, h) for s_ in range(2) for h in range(4)]

    def loads(n):
        s_, h = heads[n]
        p_ = n % 2
        t0 = s_ * S
        for i in range(2):
            r0 = 2048 + i * 512 + h * 128
            P.dma('actq', raw[p_][i][:], k.projT[r0:r0 + 128, t0:t0 + S], [], ['e3_raw%d_%d' % (p_, i)])
        P.dma('actq', V[p_][:], k.fvtm[t0:t0 + S, h * 128:(h + 1) * 128].rearrange("(j p) d -> p j d", p=128), [], ['e3_V%d' % p_])
        P.dma('actq', CK[p_][0:2, :], k.cumhl[h, 0:2, t0:t0 + S], [], ['e3_CK%d' % p_])
        P.dma('actq', CQ[p_][2:4, :], k.cumhl[h, 2:4, t0:t0 + S], [], ['e3_CQ%d' % p_])

    nbk = [0]

    def norm_block(n, i, b):
        p_ = n % 2
        j = nbk[0] % 2
        nbk[0] += 1
        cs = slice(b * 512, (b + 1) * 512)
        kr = 'e3_raw%d_%d' % (p_, i)
        P.tt('dve', sq[j][:], raw[p_][i][:, cs], raw[p_][i][:, cs], ALU.mult, [kr], ['e3_sq%d' % j])
        pz = psx if j == 0 else psn
        kz = 'e3_psx' if j == 0 else 'e3_psn'
        P.mm(pz[:], k.onesb[:], sq[j][:], True, True, ['e3_sq%d' % j, 'onesb'], [kz])
        P.act(rt[j][:], pz[:], AF.Ln, [kz], ['e3_rt%d' % j], bias=EPS, scale=1.0 / 128)
        P.act(rt[j][:], rt[j][:], AF.Exp, ['e3_rt%d' % j], ['e3_rt%d' % j], scale=-0.5)
        P.tt('dve', qn[j][:], raw[p_][i][:, cs], rt[j][:], ALU.mult, [kr, 'e3_rt%d' % j], ['e3_qn%d' % j])
        P.act(qk[p_][i][:, cs], qn[j][:], AF.Copy, ['e3_qn%d' % j, 'e3_gs'], [('e3_qk%d_%d' % (p_, i), b)], scale=gs[:, i:i + 1])

    cnt = {'it': 0, 'nq': 0}

    def attention(n, filler):
        s_, h = heads[n]
        p_ = n % 2
        t0 = s_ * S
        qT, kT = qk[p_]
        kq = [('e3_qk%d_%d' % (p_, i), b) for i in range(2) for b in range(4)]
        its = []
        for Q in range(4):
            for j in range(4 * Q + 4):
                its.append((Q, j))

        def scores(m):
            Q, j = its[m]
            r = j - 4 * Q
            c0 = max(r, 0) * 128
            i2 = (cnt['it'] + m) % 2
            ps_s = pss[i2]
            q0 = Q * 512 + c0
            P.mm(ps_s[:, c0:512], kT[:, j * 128:(j + 1) * 128], qT[:, q0:(Q + 1) * 512], True, False, kq, ['e3_pss%d' % i2])
            P.mm(ps_s[:, c0:512], CK[p_][:, j * 128:(j + 1) * 128], CQ[p_][:, q0:(Q + 1) * 512], False, True,
                 ['e3_CK%d' % p_, 'e3_CQ%d' % p_], ['e3_pss%d' % i2])

        scores(0)
        for m in range(len(its)):
            Q, j = its[m]
            last = 4 * Q + 3
            r = j - 4 * Q
            c0 = max(r, 0) * 128
            i2 = (cnt['it'] + m) % 2
            i3 = (cnt['it'] + m) % 3
            ps_s, pT = pss[i2], ptb[i3]
            ks, kp = 'e3_pss%d' % i2, 'e3_pt%d' % i3
            nq = cnt['nq']
            po, pd = pso[nq % 2], psd[nq % 2]
            ko, kd = 'e3_pso%d' % (nq % 2), 'e3_psd%d' % (nq % 2)
            P.act(pT[:, c0:512], ps_s[:, c0:512], AF.Exp, [ks, 'e3_negm'], [kp], bias=negm[:, 0:1])
            if r >= 0:
                P.tt('dve', pT[:, c0:c0 + 128], pT[:, c0:c0 + 128], k.mUf[:], ALU.mult, [kp, 'mUf'], [kp])
            if m + 1 < len(its):
                scores(m + 1)
            P.mm(po[:, c0:512], V[p_][:, j, :], pT[:, c0:512], j == 0, j == last, ['e3_V%d' % p_, kp], [ko])
            P.mm(pd[:, c0:512], k.onesb[:], pT[:, c0:512], j == 0, j == last, ['onesb', kp], [kd])
            if j == last:
                o = ot[nq % 2]
                P.act(rden[:], pd[:], AF.Ln, [kd], ['e3_rden'])
                P.act(rden[:], rden[:], AF.Exp, ['e3_rden'], ['e3_rden'], scale=-1.0)
                P.tt('dve', o[:], po[:], rden[:], ALU.mult, [ko, 'e3_rden'], ['e3_ot%d' % (nq % 2)])
                P.dma('sp', k.omixT[512 + h * 128:512 + (h + 1) * 128, t0 + Q * 512:t0 + (Q + 1) * 512], o[:],
                      ['e3_ot%d' % (nq % 2)], [('omixT', h, s_, Q)])
                cnt['nq'] += 1
            if m % 5 == 4 and filler:
                filler.pop(0)()
        while filler:
            filler.pop(0)()
        cnt['it'] += len(its)

    loads(0)
    for i in range(2):
        for b in range(4):
            norm_block(0, i, b)
    for n in range(8):
        filler = []
        if n + 1 < 8:
            loads(n + 1)
            filler = [(lambda i=i, b=b, n=n: norm_block(n + 1, i, b)) for i in range(2) for b in range(4)]
        attention(n, filler)
    P.pop()


def phase_e2a(P, k):
    P.push()
    cw = P.sb("e2a_cw", [128, 12, 4], F32)
    for kk in range(4):
        P.dma('sp', cw[:, :, kk], k.ev_conv_w[0, kk, :].rearrange("(ti p) -> p ti", p=128), [], ['e2a_cw'],
              allow_slow_non_contiguous=True)
    dg = P.sb("e2a_dg", [128, 12, 4, 128], BF16)
    for ti in range(12):
        for kk in range(4):
            P.ts('dve', dg[:, ti, kk, :], k.id32[:], cw[:, ti, kk:kk + 1], None, ALU.mult, None, ['e2a_cw', 'id32'], ['e2a_dg'])
    raw = [P.sb("e2a_raw%d" % i, [128, 3 + S], BF16) for i in range(2)]
    for i in range(2):
        P.memset('pool', raw[i][:, 0:3], 0.0, ['e2a_raw%d' % i])
    psc = [P.ps("e2a_psc%d" % i, [128, 512]) for i in range(2)]
    psn2 = [P.ps("e2a_psn%d" % i, [128, 512]) for i in range(2)]
    cs = P.sb("e2a_cs", [128, S], F32)
    sq2 = [P.sb("e2a_sq%d" % i, [128, 512], BF16) for i in range(2)]
    rt2 = [P.sb("e2a_rt%d" % i, [128, 512], F32) for i in range(2)]
    ob = [P.sb("e2a_ob%d" % i, [128, S], BF16) for i in range(2)]
    n = 0
    nb = 0
    for s in range(2):
        t0 = s * S
        for ti in range(12):
            rw = raw[n % 2]
            kr = 'e2a_raw%d' % (n % 2)
            o = ob[n % 2]
            ko = 'e2a_ob%d' % (n % 2)
            n += 1
            P.dma('sp', rw[:, 3:3 + S], k.projT[ti * 128:(ti + 1) * 128, t0:t0 + S], [], [kr])
            for b in range(4):
                pc = psc[nb % 2]
                kc_ = 'e2a_psc%d' % (nb % 2)
                nb += 1
                for kk in range(4):
                    P.mm(pc[:], dg[:, ti, kk, :], rw[:, b * 512 + kk:b * 512 + kk + 512], kk == 0, kk == 3, ['e2a_dg', kr], [kc_])
                if ti < 8:
                    P.act(cs[:, b * 512:(b + 1) * 512], pc[:], AF.Silu, [kc_], [('e2a_cs', b)])
                else:
                    P.act(o[:, b * 512:(b + 1) * 512], pc[:], AF.Silu, [kc_], [ko])
            if ti < 8:
                sc = 128 ** -0.5 if ti < 4 else 1.0
                for b in range(4):
                    c_ = slice(b * 512, (b + 1) * 512)
                    i2 = b % 2
                    sq, rt, psn = sq2[i2], rt2[i2], psn2[i2]
                    ksq, krt, kpn = 'e2a_sq%d' % i2, 'e2a_rt%d' % i2, 'e2a_psn%d' % i2
                    P.tt('dve', sq[:], cs[:, c_], cs[:, c_], ALU.mult, [('e2a_cs', b)], [ksq])
                    P.mm(psn[:], k.onesb[:], sq[:], True, True, [ksq, 'onesb'], [kpn])
                    P.act(rt[:], psn[:], AF.Ln, [kpn], [krt], bias=EPS, scale=1.0)
                    P.act(rt[:], rt[:], AF.Exp, [krt], [krt], scale=-0.5)
                    P.stt(o[:, c_], cs[:, c_], sc, rt[:], ALU.mult, ALU.mult, [('e2a_cs', b), krt], [ko])
            P.dma('sp', k.dnT[ti * 128:(ti + 1) * 128, t0:t0 + S], o[:], [ko], [('dnT', ti, s)])
    P.pop()


def phase_e2b(P, k):
    P.push()
    X = [P.ps("e2_X%d" % g, [128, 512]) for g in range(4)]
    Y = [P.ps("e2_Y%d" % g, [128, 512]) for g in range(4)]
    wT = P.sb("e2_wT", [128, 4, S], BF16)
    uu = P.sb("e2_u", [128, 4, 16, 128], BF16)
    qdT = P.sb("e2_qdT", [128, 4, S], BF16)
    qkT = P.sb("e2_qkT", [128, 4, 16, 128], BF16)
    kdec = P.sb("e2_kdec", [128, 4, 16, 128], BF16)
    glast = P.sb("e2_glast", [128, 4, 32], F32)
    oT = P.sb("e2_oT", [128, 4, S], F32)
    qin = [P.sb("e2_qin%d" % i, [128, 3, S], BF16) for i in range(2)]
    ab = P.sb("e2_ab", [128, 16, 8], F32)
    cst = P.sb("e2_cst", [128, 3, 4], F32)
    gw = P.sb("e2_gw", [128, 8, 64], F32)
    sp = P.sb("e2_sp", [128, 16, 4], F32)
    ocol = P.sb("e2_ocol", [128, 1], F32)
    P.dma('sp', cst[:, 0, :], k.ev_dn_dt_bias[0:1, :].partition_broadcast(128), [], ['e2_cst'])
    P.dma('sp', cst[:, 1, :], k.ev_dn_a_log[0:1, :].partition_broadcast(128), [], ['e2_cst'])
    P.dma('sp', ocol[:], k.ev_dn_onorm_g.rearrange("o d -> d o"), [], ['e2_ocol'], allow_slow_non_contiguous=True)
    P.act(cst[:, 2, :], cst[:, 1, :], AF.Exp, ['e2_cst'], ['e2_cst'])
    P.ts('dve', cst[:, 2, :], cst[:, 2, :], -1.0, None, ALU.mult, None, ['e2_cst'], ['e2_cst'])
    rhsd = [P.sb("e2_rhsd%d" % g, [128, 2, 128], F32) for g in range(4)]
    t1 = [P.sb("e2_t1%d" % g, [128, 256], F32) for g in range(4)]
    EE = [P.sb("e2_EE%d" % g, [128, 256], F32) for g in range(4)]
    egc = [P.sb("e2_egc%d" % g, [128, 128], F32) for g in range(4)]
    prod = [P.sb("e2_prod%d" % g, [128, 256], F32) for g in range(4)]
    pp = [[P.sb("e2_pp%d_%d" % (g, i), [128, 256], BF16) for i in range(2)] for g in range(4)]
    RR = [[P.sb("e2_R%d_%d" % (g, i), [128, 128], BF16) for i in range(2)] for g in range(4)]
    TTb = [P.sb("e2_TTb%d" % g, [128, 128], BF16) for g in range(4)]
    kbg = [P.sb("e2_kbg%d" % g, [128, 128], BF16) for g in range(4)]
    vb = [P.sb("e2_vb%d" % g, [128, 128], BF16) for g in range(4)]
    S32 = [P.sb("e2_S32_%d" % g, [128, 128], F32) for g in range(4)]
    Sb = [P.sb("e2_Sb%d" % g, [128, 128], BF16) for g in range(4)]
    vnb = [P.sb("e2_vnb%d" % g, [128, 128], BF16) for g in range(4)]
    zt = P.sb("e2_zt", [128, S], BF16)
    sq = P.sb("e2_sq", [128, 512], BF16)
    rt = P.sb("e2_rt", [128, 512], F32)
    on = P.sb("e2_on", [128, 512], F32)
    sz = P.sb("e2_sz", [128, S], BF16)
    fin = [P.sb("e2_fin%d" % i, [128, 512], BF16) for i in range(2)]
    nin = 0
    nfin = 0
    for s in range(2):
        t0 = s * S
        P.dma('sp', ab[:], k.abtm[t0:t0 + S, :].rearrange("(j p) c -> p j c", p=128), [], ['e2_ab'])
        for h in range(4):
            P.act(sp[:, :, h], ab[:, :, h], AF.Exp, ['e2_ab', 'e2_cst'], ['e2_sp'], bias=cst[:, 0, h:h + 1])
        P.act(sp[:], sp[:], AF.Ln, ['e2_sp'], ['e2_sp'], bias=1.0)
        ld = gw[:, 0, :].rearrange("p (j h) -> p j h", h=4)
        for h in range(4):
            P.ts('dve', ld[:, :, h], sp[:, :, h], cst[:, 2, h:h + 1], None, ALU.mult, None, ['e2_sp', 'e2_cst'], ['e2_gw0'])
        P.act(gw[:, 1, :].rearrange("p (j h) -> p j h", h=4), ab[:, :, 4:8], AF.Sigmoid, ['e2_ab'], ['e2_gw1'])
        P.act(gw[:, 2, :], gw[:, 1, :], AF.Ln, ['e2_gw1'], ['e2_gw2'])
        P.mm(Y[0][:, 0:64], k.ltri[:, 0, :], gw[:, 0, :], True, True, ['ltri', 'e2_gw0'], ['e2_Y0a'])
        P.mm(Y[0][:, 64:128], k.ltri[:, 1, :], gw[:, 0, :], True, True, ['ltri', 'e2_gw0'], ['e2_Y0a'])
        P.cp('dve', gw[:, 3:5, :], Y[0][:, 0:128].rearrange("p (a n) -> p a n", a=2), ['e2_Y0a'], ['e2_gw3', 'e2_gw4'])
        P.act(gw[:, 5, :], gw[:, 3, :], AF.Exp, ['e2_gw3'], ['e2_gw5'])
        P.tt('dve', gw[:, 5, :], gw[:, 5, :], gw[:, 1, :], ALU.mult, ['e2_gw5', 'e2_gw1'], ['e2_gw5'])
        P.tt('dve', gw[:, 6, :], gw[:, 4, :], gw[:, 3, :], ALU.subtract, ['e2_gw3', 'e2_gw4'], ['e2_gw6'])
        P.act(gw[:, 6, :], gw[:, 6, :], AF.Exp, ['e2_gw6'], ['e2_gw6'])
        P.tt('dve', gw[:, 7, :], gw[:, 3, :], gw[:, 2, :], ALU.add, ['e2_gw3', 'e2_gw2'], ['e2_gw7'])
        gk = ['e2_gw%d' % i for i in range(8)]
        for hp in range(2):
            hs = [2 * hp, 2 * hp + 1]
            qb = {}
            for h in hs:
                buf = qin[h % 2]
                for i in range(3):
                    P.dma('sp', buf[:, i, :], k.dnT[i * 512 + h * 128:i * 512 + (h + 1) * 128, t0:t0 + S], [], ['e2_qin%d' % (h % 2)])
                qb[h] = buf
            for t2 in range(0, 16, 2):
                ctx = []
                for ti_ in (t2, t2 + 1):
                    for h in hs:
                        g = (h % 2) * 2 + (ti_ % 2)
                        idx = ti_ * 4 + h
                        ctx.append((h, g, idx, qb[h], 'e2_qin%d' % (h % 2), ti_, slice(ti_ * 128, (ti_ + 1) * 128)))
                for (h, g, idx, qb_, kq, ti, tsl) in ctx:
                    P.ts('dve', rhsd[g][:, 0, :], k.id32[:], gw[:, 3, idx:idx + 1], None, ALU.mult, None, ['id32', 'e2_gw3'],
                         ['e2_rhsd%d' % g])
                    P.ts('dve', rhsd[g][:, 1, :], k.id32[:], gw[:, 7, idx:idx + 1], None, ALU.mult, None, ['id32', 'e2_gw7'],
                         ['e2_rhsd%d' % g])
                for (h, g, idx, qb_, kq, ti, tsl) in ctx:
                    P.mm(X[g][:, 0:256], k.ones32[:], rhsd[g][:].rearrange("p a n -> p (a n)"), True, True,
                         ['ones32', 'e2_rhsd%d' % g], ['e2_X%da' % g])
                    P.mm(Y[g][:, 0:128], qb_[:, 1, tsl], qb_[:, 0, tsl], True, True, [kq], ['e2_Y%da' % g])
                    P.mm(Y[g][:, 128:256], qb_[:, 1, tsl], qb_[:, 1, tsl], True, True, [kq], ['e2_Y%da' % g])
                for (h, g, idx, qb_, kq, ti, tsl) in ctx:
                    P.act(t1[g][:], X[g][:, 0:256], AF.Relu, ['e2_X%da' % g, 'e2_gw3'], ['e2_t1%d' % g],
                          bias=gw[:, 3, idx:idx + 1], scale=-1.0)
                    P.act(egc[g][:], X[g][:, 0:128], AF.Exp, ['e2_X%da' % g], ['e2_egc%d' % g])
                    P.act(t1[g][:], t1[g][:], AF.Exp, ['e2_t1%d' % g], ['e2_t1%d' % g], scale=-1.0)
                for (h, g, idx, qb_, kq, ti, tsl) in ctx:
                    P.tt('pool', EE[g][:], t1[g][:], k.mU[:].rearrange("p a n -> p (a n)"), ALU.mult, ['e2_t1%d' % g, 'mU'],
                         ['e2_EE%d' % g])
                    P.tt('pool', qdT[:, h, tsl], qb_[:, 0, tsl], egc[g][:], ALU.mult, [kq, 'e2_egc%d' % g], [('e2_qdT', h)])
                    P.cp('dve', glast[:, h, 2 * ti:2 * ti + 2], egc[g][:, 63:128:64], ['e2_egc%d' % g], [('e2_glast', h)])
                for (h, g, idx, qb_, kq, ti, tsl) in ctx:
                    P.tt('dve', prod[g][:], Y[g][:, 0:256], EE[g][:], ALU.mult, ['e2_Y%da' % g, 'e2_EE%d' % g], ['e2_prod%d' % g])
                for (h, g, idx, qb_, kq, ti, tsl) in ctx:
                    P.mm(Y[g][:, 0:128], qb_[:, 1, tsl], k.idb[:], True, True, [kq, 'idb'], ['e2_Y%da' % g])
                    P.mm(Y[g][:, 128:256], qb_[:, 2, tsl], k.idb[:], True, True, [kq, 'idb'], ['e2_Y%da' % g])
                for (h, g, idx, qb_, kq, ti, tsl) in ctx:
                    P.cp('pool', qkT[:, h, ti, :], prod[g][:, 0:128], ['e2_prod%d' % g], [('e2_qkT', h)])
                    P.tt('pool', RR[g][0][:], k.id32[:], prod[g][:, 128:256], ALU.subtract, ['id32', 'e2_prod%d' % g],
                         ['e2_R%d_0' % g])
                    P.cp('pool', pp[g][0][:, 0:128], prod[g][:, 128:256], ['e2_prod%d' % g], ['e2_pp%d_0' % g])
                    P.mm(X[g][:, 256:384], pp[g][0][:, 0:128], k.idb[:], True, True, ['e2_pp%d_0' % g, 'idb'], ['e2_X%db' % g])
                for (h, g, idx, qb_, kq, ti, tsl) in ctx:
                    P.cp('act', pp[g][0][:, 128:256], X[g][:, 256:384], ['e2_X%db' % g], ['e2_pp%d_0' % g])
                    P.ts('dve', kbg[g][:], Y[g][:, 0:128], gw[:, 5, idx:idx + 1], None, ALU.mult, None,
                         ['e2_Y%da' % g, 'e2_gw5'], ['e2_kbg%d' % g])
                    P.ts('dve', kdec[:, h, ti, :], Y[g][:, 0:128], gw[:, 6, idx:idx + 1], None, ALU.mult, None,
                         ['e2_Y%da' % g, 'e2_gw6'], [('e2_kdec', h)])
                    P.ts('dve', vb[g][:], Y[g][:, 128:256], gw[:, 1, idx:idx + 1], None, ALU.mult, None,
                         ['e2_Y%da' % g, 'e2_gw1'], ['e2_vb%d' % g])
                for step in range(5):
                    a, b = step % 2, (step + 1) % 2
                    for (h, g, idx, qb_, kq, ti, tsl) in ctx:
                        kpa, kpb = 'e2_pp%d_%d' % (g, a), 'e2_pp%d_%d' % (g, b)
                        if step < 4:
                            P.mm(X[g][:, 0:128], pp[g][a][:, 128:256], pp[g][a][:, 0:128], True, True, [kpa], ['e2_X%da' % g])
                        P.mm(X[g][:, 128:256], pp[g][a][:, 0:128], pp[g][a][:, 128:256], True, True, [kpa], ['e2_X%da' % g])
                    for (h, g, idx, qb_, kq, ti, tsl) in ctx:
                        kpa, kpb = 'e2_pp%d_%d' % (g, a), 'e2_pp%d_%d' % (g, b)
                        if step < 4:
                            P.cp('act', pp[g][b][:], X[g][:, 0:256], ['e2_X%da' % g], [kpb])
                        else:
                            P.cp('act', pp[g][b][:, 128:256], X[g][:, 128:256], ['e2_X%da' % g], [kpb])
                    for (h, g, idx, qb_, kq, ti, tsl) in ctx:
                        kpb = 'e2_pp%d_%d' % (g, b)
                        P.mm(Y[g][:, 256:384], pp[g][b][:, 128:256], RR[g][a][:], True, True, [kpb, 'e2_R%d_%d' % (g, a)],
                             ['e2_Y%db' % g])
                    for (h, g, idx, qb_, kq, ti, tsl) in ctx:
                        P.tt('dve', RR[g][b][:], Y[g][:, 256:384], RR[g][a][:], ALU.add, ['e2_Y%db' % g, 'e2_R%d_%d' % (g, a)],
                             ['e2_R%d_%d' % (g, b)])
                for (h, g, idx, qb_, kq, ti, tsl) in ctx:
                    P.cp('pool', TTb[g][:], RR[g][1][:], ['e2_R%d_1' % g], ['e2_TTb%d' % g])
                for (h, g, idx, qb_, kq, ti, tsl) in ctx:
                    P.mm(X[g][:, 256:384], TTb[g][:], vb[g][:], True, True, ['e2_TTb%d' % g, 'e2_vb%d' % g], ['e2_X%db' % g])
                    P.mm(X[g][:, 384:512], kbg[g][:], TTb[g][:], True, True, ['e2_TTb%d' % g, 'e2_kbg%d' % g], ['e2_X%db' % g])
                for (h, g, idx, qb_, kq, ti, tsl) in ctx:
                    P.cp('act', uu[:, h, ti, :], X[g][:, 256:384], ['e2_X%db' % g], [('e2_u', h)])
                    P.cp('act', wT[:, h, tsl], X[g][:, 384:512], ['e2_X%db' % g], [('e2_wT', h)])
        for h in range(4):
            P.memset('pool', S32[h][:], 0.0, ['e2_S32_%d' % h])
            P.memset('pool', Sb[h][:], 0.0, ['e2_Sb%d' % h])
            P.memset('pool', vnb[h][:], 0.0, ['e2_vnb%d' % h])
        for c in range(32):
            ti, r = c // 2, c % 2
            rs = slice(r * 64, r * 64 + 64)
            tsl = slice(ti * 128, (ti + 1) * 128)
            csl = slice(c * 64, (c + 1) * 64)
            for h in range(4):
                P.mm(Y[h][:, 0:128], wT[:, h, tsl], Sb[h][:], True, True, [('e2_wT', h), 'e2_Sb%d' % h], ['e2_Y%da' % h])
            for h in range(4):
                P.tt('dve', vnb[h][rs, :], uu[rs, h, ti, :], Y[h][rs, 0:128], ALU.subtract, [('e2_u', h), 'e2_Y%da' % h],
                     ['e2_vnb%d' % h])
            for h in range(4):
                P.mm(X[h][:, 0:64], Sb[h][:], qdT[:, h, csl], True, False, ['e2_Sb%d' % h, ('e2_qdT', h)], ['e2_X%da' % h])
                P.mm(X[h][:, 0:64], vnb[h][rs, :], qkT[rs, h, ti, r * 64:(r + 1) * 64], False, True,
                     ['e2_vnb%d' % h, ('e2_qkT', h)], ['e2_X%da' % h])
                P.mm(Y[h][:, 256:384], kdec[rs, h, ti, :], vnb[h][rs, :], True, True, [('e2_kdec', h), 'e2_vnb%d' % h],
                     ['e2_Y%db' % h])
            for h in range(4):
                P.cp('act', oT[:, h, csl], X[h][:, 0:64], ['e2_X%da' % h], [('e2_oT', h)])
                P.stt(S32[h][:], S32[h][:], glast[:, h, c:c + 1], Y[h][:, 256:384], ALU.mult, ALU.add,
                      ['e2_S32_%d' % h, ('e2_glast', h), 'e2_Y%db' % h], ['e2_S32_%d' % h])
                P.cp('act', Sb[h][:], S32[h][:], ['e2_S32_%d' % h], ['e2_Sb%d' % h])
        for h in range(4):
            P.dma('sp', zt[:], k.projT[1536 + h * 128:1536 + (h + 1) * 128, t0:t0 + S], [], ['e2_zt'])
            P.act(sz[:], zt[:], AF.Silu, ['e2_zt'], ['e2_sz'])
            for b in range(4):
                c_ = slice(b * 512, (b + 1) * 512)
                P.tt('dve', sq[:], oT[:, h, c_], oT[:, h, c_], ALU.mult, [('e2_oT', h)], ['e2_sq'])
                P.mm(X[b][:], k.onesb[:], sq[:], True, True, ['e2_sq', 'onesb'], ['e2_X%da' % b, 'e2_X%db' % b])
                P.act(rt[:], X[b][:], AF.Ln, ['e2_X%da' % b, 'e2_X%db' % b], ['e2_rt'], bias=EPS, scale=1.0 / 128)
                P.act(rt[:], rt[:], AF.Exp, ['e2_rt'], ['e2_rt'], scale=-0.5)
                P.tt('dve', on[:], oT[:, h, c_], rt[:], ALU.mult, [('e2_oT', h), 'e2_rt'], ['e2_on'])
                f = fin[nfin % 2]
                kf = 'e2_fin%d' % (nfin % 2)
                nfin += 1
                P.stt(f[:], on[:], ocol[:, 0:1], sz[:, c_], ALU.mult, ALU.mult, ['e2_on', 'e2_ocol', 'e2_sz'], [kf])
                P.dma('sp', k.omixT[h * 128:(h + 1) * 128, t0 + b * 512:t0 + (b + 1) * 512], f[:], [kf], [('omixT_dn', h, s, b)])
    P.pop()


def phase_outproj(P, k, w_ap, l, xsrc, xdst, pfx):
    P.push()
    W = P.sb(pfx + "_W", [128, 8, 1024], BF16)
    load_w_bf16(P, W, w_ap, pfx + '_W')
    gt = []
    for b in range(2):
        g = P.sb(pfx + "_gt%d" % b, [128, 1024], F32)
        o = OFF['gt1']
        P.dma('sp', g[:], k.modrows[l, b:b + 1, o:o + 1024].partition_broadcast(128), [], [pfx + '_gt%d' % b])
        gt.append(g)
    NB = 4
    om = [P.sb(pfx + "_om%d" % i, [128, 8, 128], BF16) for i in range(NB)]
    xt = [P.sb(pfx + "_xt%d" % i, [128, 1024], F32) for i in range(NB)]
    tmp = [P.sb(pfx + "_tmp%d" % i, [128, 1024], F32) for i in range(NB)]
    ps = [[P.ps(pfx + "_ps%d_%d" % (i, hf), [128, 512]) for hf in range(2)] for i in range(NB)]
    for ti in range(NT):
        j = ti % NB
        b = ti // 16
        tsl = slice(ti * 128, (ti + 1) * 128)
        ko, kx, kt = pfx + '_om%d' % j, pfx + '_xt%d' % j, pfx + '_tmp%d' % j
        P.dma('sp', om[j][:], k.omixT[:, tsl].rearrange("(kc p) t -> p kc t", p=128), [], [ko])
        P.dma('actq', xt[j][:], xsrc[tsl, :], [], [kx])
        for hf in range(2):
            kp = pfx + '_ps%d_%d' % (j, hf)
            for kc in range(8):
                P.mm(ps[j][hf][:], om[j][:, kc, :], W[:, kc, hf * 512:(hf + 1) * 512], kc == 0, kc == 7, [ko, pfx + '_W'], [kp])
            P.tt('dve', tmp[j][:, hf * 512:(hf + 1) * 512], ps[j][hf][:], gt[b][:, hf * 512:(hf + 1) * 512], ALU.mult,
                 [kp, pfx + '_gt%d' % b], [(kt, hf)])
        P.tt('pool', tmp[j][:], tmp[j][:], xt[j][:], ALU.add, [(kt, 0), (kt, 1), kx], [(kt, 0), (kt, 1)])
        P.dma('actq', xdst[tsl, :], tmp[j][:], [(kt, 0), (kt, 1)], [('xdst', ti)])
    P.pop()


def phase_moe(P, k, l, xsrc, xdst, pfx):
    for s in range(2):
        P.push()
        t0 = s * S
        hT = P.sb(pfx + "_hT", [128, 8, S], BF16)
        gm = P.sb(pfx + "_gm", [128, 16, 16], F32)
        P.push()
        nm = Normer(P, k, l, 2, xsrc, True)
        rw = P.sb(pfx + "_rw", [128, 8, 16], F32)
        P.dma('sp', rw[:], k.router_w.rearrange("(kc p) e -> p kc e", p=128), [], [pfx + '_rw'])
        rb = P.sb(pfx + "_rb", [128, 16], F32)
        P.dma('sp', rb[:], k.router_b.rearrange("(o e) -> o e", o=1).partition_broadcast(128), [], [pfx + '_rb'])
        h32 = [P.sb(pfx + "_h32_%d" % i, [128, 8, 128], F32) for i in range(2)]
        plg = P.ps(pfx + "_plg", [128, 16, 16])
        for tl in range(16):
            ti = s * 16 + tl
            j = tl % 2
            hk = [(pfx + '_h32_%d' % j, kc) for kc in range(8)]
            nm.tile(ti, lambda kc, j=j: h32[j][:, kc, :], hk)
            P.cp('pool', hT[:, :, tl * 128:(tl + 1) * 128], h32[j][:], hk, [(pfx + '_hT', tl)])
            for kc in range(8):
                P.mm(plg[:, tl, :], h32[j][:, kc, :], rw[:, kc, :], kc == 0, kc == 7, hk + [pfx + '_rw'], [pfx + '_plg'])
        L = P.sb(pfx + "_L", [128, 16, 16], F32)
        E_ = P.sb(pfx + "_E", [128, 16, 16], F32)
        pr = P.sb(pfx + "_pr", [128, 16, 16], F32)
        sel = P.sb(pfx + "_sel", [128, 16, 16], F32)
        ps6 = P.sb(pfx + "_ps6", [128, 64, 6], F32)
        gs = P.sb(pfx + "_gs", [128, 16, 4], F32)
        oh = P.sb(pfx + "_oh", [128, 16, 4], F32)
        msk = P.sb(pfx + "_msk", [128, 16, 16], F32)
        ing = P.sb(pfx + "_ing", [128, 16, 4], F32)
        ing2 = P.sb(pfx + "_ing2", [128, 16, 4], F32)
        oh1 = P.sb(pfx + "_oh1", [128, 16, 4], F32)
        lm = P.sb(pfx + "_lm", [128, 16, 4], F32)
        col = P.sb(pfx + "_col", [128, 8, 16], F32)
        R_ = pfx + '_r'

        def bc(ap, shape):
            return ap.to_broadcast(shape)

        def dv(fn, rd, wr):
            P.op('dve', fn, [R_ + x for x in rd], [R_ + x for x in wr])
        P.cp('dve', L[:], plg[:], [pfx + '_plg'], [R_ + 'L'])
        dv(lambda e: e.tensor_reduce(col[:, 0, :], L[:], AX.X, ALU.max), ['L'], ['c0'])
        dv(lambda e: e.tensor_tensor(L[:], L[:], bc(col[:, 0, :].unsqueeze(2), [128, 16, 16]), ALU.subtract), ['L', 'c0'], ['L'])
        P.act(E_[:], L[:], AF.Exp, [R_ + 'L'], [R_ + 'E'])
        dv(lambda e: e.tensor_reduce(col[:, 1, :], E_[:], AX.X, ALU.add), ['E'], ['c1'])
        dv(lambda e: e.reciprocal(col[:, 1, :], col[:, 1, :]), ['c1'], ['c1'])
        dv(lambda e: e.tensor_tensor(pr[:], E_[:], bc(col[:, 1, :].unsqueeze(2), [128, 16, 16]), ALU.mult), ['E', 'c1'], ['pr'])
        dv(lambda e: e.tensor_tensor(sel[:], pr[:], bc(rb[:].unsqueeze(1), [128, 16, 16]), ALU.add), ['pr'], ['sel'])
        P.readers.setdefault(pfx + '_rb', [])
        s4 = sel[:].rearrange("p t (g j) -> p (t g) j", j=4)
        dv(lambda e: e.tensor_tensor(ps6[:, :, 0:3], bc(s4[:, :, 0:1], [128, 64, 3]), s4[:, :, 1:4], ALU.add), ['sel'], ['ps6a'])
        dv(lambda e: e.tensor_tensor(ps6[:, :, 3:5], bc(s4[:, :, 1:2], [128, 64, 2]), s4[:, :, 2:4], ALU.add), ['sel'], ['ps6b'])
        dv(lambda e: e.tensor_tensor(ps6[:, :, 5:6], s4[:, :, 2:3], s4[:, :, 3:4], ALU.add), ['sel'], ['ps6c'])
        dv(lambda e: e.tensor_reduce(gs[:].rearrange("p t g -> p (t g)"), ps6[:], AX.X, ALU.max), ['ps6a', 'ps6b', 'ps6c'], ['gs'])
        dv(lambda e: e.tensor_reduce(col[:, 2, :], gs[:], AX.X, ALU.max), ['gs'], ['c2'])
        dv(lambda e: e.tensor_tensor(oh[:], gs[:], bc(col[:, 2, :].unsqueeze(2), [128, 16, 4]), ALU.is_equal), ['gs', 'c2'], ['oh'])
        dv(lambda e: e.tensor_tensor(msk[:].rearrange("p t (g j) -> p (t g) j", j=4), s4,
                                     bc(oh[:].rearrange("p t g -> p (t g)").unsqueeze(2), [128, 64, 4]), ALU.mult), ['sel', 'oh'], ['msk'])
        dv(lambda e: e.tensor_reduce(ing[:], msk[:].rearrange("p t (g j) -> p t j g", j=4), AX.X, ALU.add), ['msk'], ['ing'])
        dv(lambda e: e.tensor_reduce(col[:, 3, :], ing[:], AX.X, ALU.max), ['ing'], ['c3'])
        dv(lambda e: e.tensor_tensor(oh1[:], ing[:], bc(col[:, 3, :].unsqueeze(2), [128, 16, 4]), ALU.is_equal), ['ing', 'c3'], ['oh1'])
        dv(lambda e: e.scalar_tensor_tensor(ing2[:], oh1[:], -1.0e9, ing[:], ALU.mult, ALU.add), ['oh1', 'ing'], ['ing2'])
        dv(lambda e: e.tensor_reduce(col[:, 4, :], ing2[:], AX.X, ALU.max), ['ing2'], ['c4'])
        dv(lambda e: e.tensor_tensor(lm[:], ing2[:], bc(col[:, 4, :].unsqueeze(2), [128, 16, 4]), ALU.is_equal), ['ing2', 'c4'], ['lm'])
        dv(lambda e: e.tensor_tensor(lm[:], lm[:], oh1[:], ALU.add), ['lm', 'oh1'], ['lm'])
        gm4 = gm[:].rearrange("p t (g j) -> p t g j", j=4)
        for g in range(4):
            dv(lambda e, g=g: e.tensor_tensor(gm4[:, :, g, :], lm[:], bc(oh[:, :, g:g + 1], [128, 16, 4]), ALU.mult), ['lm', 'oh'],
               ['gm%d' % g])
        gmk = ['gm%d' % g for g in range(4)]
        dv(lambda e: e.tensor_tensor(gm[:], gm[:], pr[:], ALU.mult), gmk + ['pr'], ['gm'])
        dv(lambda e: e.tensor_reduce(col[:, 5, :], gm[:], AX.X, ALU.add), ['gm'], ['c5'])
        dv(lambda e: e.reciprocal(col[:, 5, :], col[:, 5, :]), ['c5'], ['c5'])
        dv(lambda e: e.tensor_tensor(gm[:], gm[:], bc(col[:, 5, :].unsqueeze(2), [128, 16, 16]), ALU.mult), ['gm', 'c5'], ['gm'])
        if getattr(k, 'dbg_gates', None) is not None and l == 0:
            P.dma('sp', k.dbg_gates[s], gm[:], [R_ + 'gm'], [('dbg_gates', s)])
        P.pop()
        acc = P.sb(pfx + "_acc", [128, 16, 1024], F32)
        wg = [P.sb(pfx + "_wg%d" % i, [128, 8, 256], BF16) for i in range(2)]
        wu = [P.sb(pfx + "_wu%d" % i, [128, 8, 256], BF16) for i in range(2)]
        wd = [P.sb(pfx + "_wd%d" % i, [128, 2, 1024], BF16) for i in range(2)]
        actT = [P.sb(pfx + "_actT%d" % i, [128, 2, S], BF16) for i in range(2)]
        sg = [P.sb(pfx + "_sg%d" % i, [128, 512], F32) for i in range(2)]
        psg = [P.ps(pfx + "_psg%d" % i, [128, 512]) for i in range(2)]
        psu = [P.ps(pfx + "_psu%d" % i, [128, 512]) for i in range(2)]
        psy = [P.ps(pfx + "_psy%d" % i, [128, 512]) for i in range(4)]
        hk = [(pfx + '_hT', tl) for tl in range(16)]
        st_ = {'it': 0, 'ny': 0}

        def down_unit(e_, tl, half):
            j = e_ % 2
            blk = tl // 4
            tsl = slice(tl * 128, (tl + 1) * 128)
            iy = st_['ny'] % 4
            st_['ny'] += 1
            py = psy[iy]
            ky = pfx + '_psy%d' % iy
            for c in range(2):
                P.mm(py[:], actT[j][:, c, tsl], wd[j][:, c, half * 512:(half + 1) * 512], c == 0, c == 1,
                     [(pfx + '_actT%d' % j, blk), pfx + '_wd%d' % j], [ky])
            dst = acc[:, tl, half * 512:(half + 1) * 512]
            ka = (pfx + '_acc', tl, half)
            gcol = gm[:, tl, e_:e_ + 1]
            if e_ == 0:
                P.ts('dve', dst, py[:], gcol, None, ALU.mult, None, [ky, pfx + '_rgm'], [ka])
            else:
                P.stt(dst, py[:], gcol, dst, ALU.mult, ALU.add, [ky, ka, pfx + '_rgm'], [ka])

        for e_ in range(17):
            j = e_ % 2
            pend = [(e_ - 1, tl, half) for tl in range(16) for half in range(2)] if e_ > 0 else []
            if e_ == 16:
                for u_ in pend:
                    down_unit(*u_)
                break
            for kc in range(8):
                P.dma('poolq', wg[j][:, kc, :], k.moe_w_gate[l, e_, kc * 128:(kc + 1) * 128, :], [], [pfx + '_wg%d' % j])
                P.dma('poolq', wu[j][:, kc, :], k.moe_w_up[l, e_, kc * 128:(kc + 1) * 128, :], [], [pfx + '_wu%d' % j])
            for c in range(2):
                P.dma('poolq', wd[j][:, c, :], k.moe_w_down[l, e_, c * 128:(c + 1) * 128, :], [], [pfx + '_wd%d' % j])
            for blk in range(4):
                bs = slice(blk * 512, (blk + 1) * 512)
                hkb = hk[blk * 4:(blk + 1) * 4]
                for hf in range(2):
                    i2 = st_['it'] % 2
                    st_['it'] += 1
                    kg, ku = pfx + '_psg%d' % i2, pfx + '_psu%d' % i2
                    for kc in range(8):
                        P.mm(psg[i2][:], wg[j][:, kc, hf * 128:(hf + 1) * 128], hT[:, kc, bs], kc == 0, kc == 7,
                             [pfx + '_wg%d' % j] + hkb, [kg])
                        if kc % 4 == 3 and pend:
                            down_unit(*pend.pop(0))
                    for kc in range(8):
                        P.mm(psu[i2][:], wu[j][:, kc, hf * 128:(hf + 1) * 128], hT[:, kc, bs], kc == 0, kc == 7,
                             [pfx + '_wu%d' % j] + hkb, [ku])
                        if kc % 4 == 3 and pend:
                            down_unit(*pend.pop(0))
                    P.act(sg[i2][:], psg[i2][:], AF.Silu, [kg], [pfx + '_sg%d' % i2])
                    P.tt('dve', actT[j][:, hf, bs], psu[i2][:], sg[i2][:], ALU.mult, [ku, pfx + '_sg%d' % i2],
                         [(pfx + '_actT%d' % j, blk)])
            assert not pend
        gt = P.sb(pfx + "_gt", [128, 1024], F32)
        o = OFF['gt2']
        P.dma('sp', gt[:], k.modrows[l, s:s + 1, o:o + 1024].partition_broadcast(128), [], [pfx + '_gt'])
        xt = [P.sb(pfx + "_xt%d" % i, [128, 1024], F32) for i in range(2)]
        for tl in range(16):
            j = tl % 2
            tsl = slice(t0 + tl * 128, t0 + (tl + 1) * 128)
            kx = pfx + '_xt%d' % j
            ka = [(pfx + '_acc', tl, 0), (pfx + '_acc', tl, 1)]
            P.dma('actq', xt[j][:], xsrc[tsl, :], [], [kx])
            P.tt('dve', acc[:, tl, :], acc[:, tl, :], gt[:], ALU.mult, ka + [pfx + '_gt'], ka)
            P.tt('pool', xt[j][:], xt[j][:], acc[:, tl, :], ALU.add, ka + [kx], [kx])
            P.dma('sp', xdst[tsl, :], xt[j][:], [kx], [('xdst', tl)])
        P.pop()


def phase_o1(P, k, xsrc):
    P.push()
    W = P.sb("o1_W", [128, 8, 2048], BF16)
    load_w_bf16(P, W, k.od_w_in[0], 'o1_W')
    nm = Normer(P, k, 1, 1, xsrc, False)
    hT = [P.sb("o1_hT%d" % i, [128, 8, 512], BF16) for i in range(2)]
    psf = [P.ps("o1_psf%d" % i, [128, 512]) for i in range(4)]
    psv = P.ps("o1_psv", [128, 512])
    stg = [P.sb("o1_stg%d" % i, [128, 512], BF16) for i in range(4)]
    stv = [P.sb("o1_stv%d" % i, [128, 512], BF16) for i in range(2)]
    nst = [0]

    def norm_a(tb, j):
        nm.tile_a(tb * 4 + j)

    def norm_b(tb, j):
        h = hT[tb % 2]
        hk = [('o1_hT%d' % (tb % 2), kc) for kc in range(8)]
        nm.tile_b(tb * 4 + j, lambda kc, h=h, j=j: h[:, kc, j * 128:(j + 1) * 128], hk)

    def fillers(tb):
        fs = [lambda: norm_a(tb, 0)]
        for j in range(1, 4):
            fs.append(lambda j=j: (norm_b(tb, j - 1), norm_a(tb, j)))
        fs.append(lambda: norm_b(tb, 3))
        return fs

    def units(tb):
        h = hT[tb % 2]
        hk = [('o1_hT%d' % (tb % 2), kc) for kc in range(8)]
        us = []

        def ftile(f):
            ib = f % 4
            ps = psf[ib]
            for kc in range(8):
                P.mm(ps[:], W[:, kc, f * 128:(f + 1) * 128], h[:, kc, :], kc == 0, kc == 7, ['o1_W'] + hk, ['o1_psf%d' % ib])
            sg = stg[nst[0] % 4]
            sk = 'o1_stg%d' % (nst[0] % 4)
            nst[0] += 1
            P.cp('act' if ib % 2 == 0 else 'dve', sg[:], ps[:], ['o1_psf%d' % ib], [sk])
            P.dma('sp', k.proj1T[f * 128:(f + 1) * 128, tb * 512:(tb + 1) * 512], sg[:], [sk], [('proj1T', f, tb)])

        def svtile(j):
            for kc in range(8):
                P.mm(psv[:], h[:, kc, j * 128:(j + 1) * 128], W[:, kc, 1536:2048], kc == 0, kc == 7, ['o1_W'] + hk, ['o1_psv'])
            sv = stv[j % 2]
            P.cp('act', sv[:], psv[:], ['o1_psv'], ['o1_stv%d' % (j % 2)])
            t0 = tb * 512 + j * 128
            P.dma('sp', k.svtm[t0:t0 + 128, :], sv[:], ['o1_stv%d' % (j % 2)], [('svtm', tb, j)])

        for f in range(12):
            us.append(lambda f=f: ftile(f))
        for j in range(4):
            us.append(lambda j=j: svtile(j))
        return us

    for f_ in fillers(0):
        f_()
    for tb in range(8):
        us = units(tb)
        fill = fillers(tb + 1) if tb + 1 < 8 else []
        for i, u_ in enumerate(us):
            u_()
            if fill and i % 3 == 1:
                fill.pop(0)()
        while fill:
            fill.pop(0)()
    P.pop()


def phase_o2(P, k):
    P.push()
    cw = P.sb("o2_cw", [128, 4, 4], F32)
    for kk in range(4):
        P.dma('sp', cw[:, :, kk], k.od_conv_w[0, kk, :].rearrange("(bl p) -> p bl", p=128), [], ['o2_cw'], allow_slow_non_contiguous=True)
    cols = P.sb("o2_cols", [128, 5, 4], F32)
    for i, ap in enumerate([k.od_conv_b, k.od_lru_br, k.od_lru_bi, k.od_lru_lambda]):
        P.dma('sp', cols[:, i, :], ap[0, :].rearrange("(bl p) -> p bl", p=128), [], ['o2_cols'], allow_slow_non_contiguous=True)
    P.act(cols[:, 4, :], cols[:, 3, :], AF.Exp, ['o2_cols'], ['o2_cols'], scale=-1.0)
    P.act(cols[:, 4, :], cols[:, 4, :], AF.Ln, ['o2_cols'], ['o2_cols'], bias=1.0)
    P.ts('dve', cols[:, 4, :], cols[:, 4, :], -8.0, None, ALU.mult, None, ['o2_cols'], ['o2_cols'])
    dg = P.sb("o2_dg", [128, 4, 4, 128], BF16)
    for bl in range(4):
        for kk in range(4):
            P.ts('dve', dg[:, bl, kk, :], k.id32[:], cw[:, bl, kk:kk + 1], None, ALU.mult, None, ['o2_cw', 'id32'], ['o2_dg'])
    wr = P.sb("o2_wr", [128, 4, 128], BF16)
    wi = P.sb("o2_wi", [128, 4, 128], BF16)
    for bl in range(4):
        P.dma('poolq', wr[:, bl, :], k.od_lru_wr[0, bl], [], ['o2_wr'])
        P.dma('poolq', wi[:, bl, :], k.od_lru_wi[0, bl], [], ['o2_wi'])
    raw = [P.sb("o2_raw%d" % i, [128, 3 + S], BF16) for i in range(2)]
    for i in range(2):
        P.memset('pool', raw[i][:, 0:3], 0.0, ['o2_raw%d' % i])
    lg = [P.sb("o2_lg%d" % i, [128, S], BF16) for i in range(2)]
    xc_2 = [P.sb("o2_xc_%d" % i, [128, S], F32) for i in range(2)]
    xcb_2 = [P.sb("o2_xcb_%d" % i, [128, S], BF16) for i in range(2)]
    aa_2 = [P.sb("o2_a_%d" % i, [128, S], F32) for i in range(2)]
    bb_2 = [P.sb("o2_b_%d" % i, [128, S], F32) for i in range(2)]
    ig_2 = [P.sb("o2_ig_%d" % i, [128, S], F32) for i in range(2)]
    hh_2 = [P.sb("o2_h_%d" % i, [128, S], F32) for i in range(2)]
    gl_2 = [P.sb("o2_gl_%d" % i, [128, S], F32) for i in range(2)]
    ob = [P.sb("o2_ob%d" % i, [128, S], BF16) for i in range(2)]
    psc = [P.ps("o2_psc%d" % i, [128, 512]) for i in range(2)]
    psr = [P.ps("o2_psr%d" % i, [128, 512]) for i in range(2)]
    psi = [P.ps("o2_psi%d" % i, [128, 512]) for i in range(2)]
    ri_2 = [P.sb("o2_ri_%d" % i, [128, 2, S], F32) for i in range(2)]
    n = 0
    for s in range(2):
        t0 = s * S
        for bl in range(4):
            j = n % 2
            n += 1
            kr, kl, ko = 'o2_raw%d' % j, 'o2_lg%d' % j, 'o2_ob%d' % j
            xc, xcb, aa, bb, ig, hh, gl, ri = xc_2[j], xcb_2[j], aa_2[j], bb_2[j], ig_2[j], hh_2[j], gl_2[j], ri_2[j]
            P.dma('sp', raw[j][:, 3:3 + S], k.proj1T[bl * 128:(bl + 1) * 128, t0:t0 + S], [], [kr])
            P.dma('sp', lg[j][:], k.proj1T[512 + bl * 128:512 + (bl + 1) * 128, t0:t0 + S], [], [kl])
            allb = lambda nm_: [(nm_, b) for b in range(4)]
            for b in range(4):
                c_ = slice(b * 512, (b + 1) * 512)
                pc = psc[b % 2]
                kc_ = 'o2_psc%d' % (b % 2)
                for kk in range(4):
                    P.mm(pc[:], dg[:, bl, kk, :], raw[j][:, b * 512 + kk:b * 512 + kk + 512], kk == 0, kk == 3, ['o2_dg', kr], [kc_])
                P.ts('dve', xc[:, c_], pc[:], cols[:, 0, bl:bl + 1], None, ALU.add, None, [kc_, 'o2_cols'], [('o2_xc%d' % j, b)])
                P.cp('pool', xcb[:, c_], xc[:, c_], [('o2_xc%d' % j, b)], [('o2_xcb%d' % j, b)])
                pr_, pi_ = psr[b % 2], psi[b % 2]
                P.mm(pr_[:], wr[:, bl, :], xcb[:, c_], True, True, ['o2_wr', ('o2_xcb%d' % j, b)], ['o2_psr%d' % (b % 2)])
                P.mm(pi_[:], wi[:, bl, :], xcb[:, c_], True, True, ['o2_wi', ('o2_xcb%d' % j, b)], ['o2_psi%d' % (b % 2)])
                P.ts('dve', ri[:, 0, c_], pr_[:], cols[:, 1, bl:bl + 1], None, ALU.add, None, ['o2_psr%d' % (b % 2), 'o2_cols'],
                     [('o2_r%d' % j, b)])
                P.ts('dve', ri[:, 1, c_], pi_[:], cols[:, 2, bl:bl + 1], None, ALU.add, None, ['o2_psi%d' % (b % 2), 'o2_cols'],
                     [('o2_i%d' % j, b)])
            P.act(ri[:], ri[:], AF.Sigmoid, allb('o2_r%d' % j) + allb('o2_i%d' % j), ['o2_ri%d' % j])
            P.act(aa[:], ri[:, 0, :], AF.Exp, ['o2_ri%d' % j, 'o2_cols'], ['o2_a%d' % j], scale=cols[:, 4, bl:bl + 1])
            P.tt('dve', bb[:], aa[:], aa[:], ALU.mult, ['o2_a%d' % j], ['o2_b%d' % j])
            P.act(bb[:], bb[:], AF.Sqrt, ['o2_b%d' % j], ['o2_b%d' % j], bias=1.0, scale=-1.0)
            P.tt('pool', ig[:], ri[:, 1, :], xc[:], ALU.mult, ['o2_ri%d' % j] + allb('o2_xc%d' % j), ['o2_ig%d' % j])
            P.tt('dve', bb[:], bb[:], ig[:], ALU.mult, ['o2_b%d' % j, 'o2_ig%d' % j], ['o2_b%d' % j])
            P.act(gl[:], lg[j][:], AF.Gelu_apprx_tanh, [kl], ['o2_gl%d' % j])
            P.op('dve', lambda e, hh=hh, aa=aa, bb=bb: e.tensor_tensor_scan(hh[:], aa[:], bb[:], 0.0, ALU.mult, ALU.add), ['o2_a%d' % j, 'o2_b%d' % j], ['o2_h%d' % j])
            P.tt('dve', ob[j][:], hh[:], gl[:], ALU.mult, ['o2_h%d' % j, 'o2_gl%d' % j], [ko])
            P.dma('sp', k.omixT[bl * 128:(bl + 1) * 128, t0:t0 + S], ob[j][:], [ko], [('omixT_lru', bl, s)])
    P.pop()


def phase_o3(P, k):
    P.push()
    wsT = P.sb("o3_wsT", [128, 4, 128], BF16)
    wtmp = P.sb("o3_wtmp", [128, 4, 128], F32)
    pw = P.ps("o3_pw", [128, 4, 128])
    P.dma('sp', wtmp[:], k.od_sgu_w[0].rearrange("g t s -> t g s"), [], ['o3_wtmp'])
    for g in range(4):
        P.mm(pw[:, g, :], wtmp[:, g, :], k.id32[:], True, True, ['o3_wtmp', 'id32'], ['o3_pw'])
    P.tt('dve', wsT[:], pw[:], k.mUf[:].unsqueeze(1).to_broadcast([128, 4, 128]), ALU.mult, ['o3_pw', 'mUf'], ['o3_wsT'])
    gb = P.sb("o3_gb", [128, 512], F32)
    bsb = P.sb("o3_bsb", [128, 512], F32)
    P.dma('sp', gb[:], k.od_sgu_norm_g[0:1, :].partition_broadcast(128), [], ['o3_gb'])
    P.dma('sp', bsb[:], k.od_sgu_b[0:1].rearrange("o g t -> o (g t)").partition_broadcast(128), [], ['o3_bsb'])
    G4 = 4
    sv = [P.sb("o3_sv%d" % i, [128, 512], BF16) for i in range(G4)]
    su = [P.sb("o3_su%d" % i, [128, 4, 128], BF16) for i in range(G4)]
    vg = [P.sb("o3_vg%d" % i, [128, 512], F32) for i in range(G4)]
    ug = [P.sb("o3_ug%d" % i, [128, 512], F32) for i in range(G4)]
    junk = P.sb("o3_junk", [128, 512], BF16)
    st = P.sb("o3_st", [128, 3, G4], F32)
    vn = [P.sb("o3_vn%d" % i, [128, 512], F32) for i in range(2)]
    vb = [P.sb("o3_vb%d" % i, [128, 512], BF16) for i in range(2)]
    mx = [P.sb("o3_mx%d" % i, [128, 512], F32) for i in range(2)]
    ob = [P.sb("o3_ob%d" % i, [128, 4, 128], BF16) for i in range(2)]
    pm = [P.ps("o3_pm%d" % i, [128, 4, 128]) for i in range(2)]
    for grp in range(NT // G4):
        for t in range(G4):
            ti = grp * G4 + t
            tsl = slice(ti * 128, (ti + 1) * 128)
            P.dma('sp', sv[t][:], k.svtm[tsl, :], [], ['o3_sv%d' % t])
            P.dma('sp', su[t][:], k.proj1T[1024:1536, tsl].rearrange("(g p) t -> p g t", p=128), [], ['o3_su%d' % t])
        for t in range(G4):
            P.act(vg[t][:], sv[t][:], AF.Gelu_apprx_tanh, ['o3_sv%d' % t], ['o3_vg%d' % t])
            P.act(ug[t][:], su[t][:].rearrange("p g t -> p (g t)"), AF.Gelu_apprx_tanh, ['o3_su%d' % t], ['o3_ug%d' % t])
        for t in range(G4):
            P.act(junk[:], vg[t][:], AF.Square, ['o3_vg%d' % t], ['o3_junk', 'o3_st'], accum_out=st[:, 0, t:t + 1])
        P.act(st[:, 1, :], st[:, 0, :], AF.Sqrt, ['o3_st'], ['o3_st'], bias=EPS, scale=1.0 / 512)
        P.recip(st[:, 2, :], st[:, 1, :], ['o3_st'], ['o3_st'])
        for t in range(G4):
            ti = grp * G4 + t
            j = ti % 2
            tsl = slice(ti * 128, (ti + 1) * 128)
            ko, kp = 'o3_ob%d' % j, 'o3_pm%d' % j
            P.act(vn[j][:], vg[t][:], AF.Copy, ['o3_vg%d' % t, 'o3_st'], ['o3_vn%d' % j], scale=st[:, 2, t:t + 1])
            P.tt('dve', vb[j][:], vn[j][:], gb[:], ALU.mult, ['o3_vn%d' % j, 'o3_gb'], ['o3_vb%d' % j])
            for g in range(4):
                P.mm(pm[j][:, g, :], vb[j][:, g * 128:(g + 1) * 128], wsT[:, g, :], True, True, ['o3_vb%d' % j, 'o3_wsT'], [kp])
            P.tt('dve', mx[j][:], pm[j][:].rearrange("p g t -> p (g t)"), bsb[:], ALU.add, [kp, 'o3_bsb'], ['o3_mx%d' % j])
            P.tt('dve', ob[j][:].rearrange("p g t -> p (g t)"), ug[t][:], mx[j][:], ALU.mult, ['o3_ug%d' % t, 'o3_mx%d' % j], [ko])
            P.dma('sp', k.omixT[512:1024, tsl].rearrange("(g p) t -> p g t", p=128), ob[j][:], [ko], [('omixT_sgu', ti)])
    P.pop()


def kernel(**inputs):
    n = 8
    nc, _ = build()
    shared = {}
    in_maps = []
    for core in range(n):
        m = {}
        for name, shape in IN_SPECS:
            if name == "x":
                a = np.ascontiguousarray(inputs["x"][2 * core:2 * core + 2], dtype=np.float32).reshape(T, D)
            elif name == "c":
                a = np.ascontiguousarray(inputs["c"][2 * core:2 * core + 2], dtype=np.float32)
            else:
                if name not in shared:
                    shared[name] = np.ascontiguousarray(inputs[name], dtype=np.float32).reshape(shape)
                a = shared[name]
            m[name] = a
        in_maps.append(m)
    res = run_bass_kernel_spmd(nc, in_maps, core_ids=list(range(n)))
    out = np.concatenate([np.asarray(res.results[c]["out"], dtype=np.float32).reshape(2, S, D) for c in range(n)], axis=0)
    return out
```

```python
import numpy as np
from contextlib import ExitStack
import concourse.bass as bass
import concourse.mybir as mybir
from concourse.bass_utils import run_bass_kernel_spmd

F32 = mybir.dt.float32
BF16 = mybir.dt.bfloat16
AF = mybir.ActivationFunctionType
ALU = mybir.AluOpType
AX = mybir.AxisListType


class Prog:
    PHYS = ['pe', 'act', 'dve', 'pool', 'sp']
    QUEUES = ['pe', 'act', 'dve', 'pool', 'sp', 'poolq', 'actq']
    ISSUER = {'pe': 'pe', 'act': 'act', 'dve': 'dve', 'pool': 'pool', 'sp': 'sp', 'poolq': 'pool', 'actq': 'act'}
    DMAQ = ('sp', 'poolq', 'actq')

    NSLOT = 8

    def __init__(self, nc):
        self.nc = nc
        self.es = ExitStack()
        self.root = self.es
        self.streams = {e: [] for e in self.PHYS}
        self.nsem = 0
        self.sems = {}
        self.count = {}
        self.ndma = {q: 0 for q in self.DMAQ}
        self.waited = {}
        self.lastw = {}
        self.readers = {}
        self._new_sems()
        self.n_ops = 0
        self.n_waits = 0

    def _semkeys(self):
        ks = []
        for q in self.QUEUES:
            if q in self.DMAQ:
                ks += [(q, i) for i in range(self.NSLOT)]
            else:
                ks.append(q)
        return ks

    def _new_sems(self):
        for sk in self._semkeys():
            nm = sk if isinstance(sk, str) else "%s%d" % sk
            self.sems[sk] = self.root.enter_context(self.nc.semaphore("s%s_%d" % (nm, self.nsem)))
            self.count[sk] = 0
        self.nsem += 1
        self.waited = {e: {sk: 0 for sk in self._semkeys()} for e in self.PHYS}
        self.lastw = {}
        self.readers = {}

    def sb(self, name, shape, dtype):
        self.uid = getattr(self, 'uid', 0) + 1
        return self.es.enter_context(self.nc.sbuf_tensor("%s_u%d" % (name, self.uid), list(shape), dtype))

    def ps(self, name, shape, dtype=F32):
        self.uid = getattr(self, 'uid', 0) + 1
        return self.es.enter_context(self.nc.psum_tensor("%s_u%d" % (name, self.uid), list(shape), dtype))

    def _inc(self, sk):
        return 1 if isinstance(sk, str) else 16

    def _need(self, phys, sk, idx):
        if self.waited[phys][sk] >= idx:
            return
        self.waited[phys][sk] = idx
        self.streams[phys].append(('wait', self.sems[sk], idx * self._inc(sk)))
        self.n_waits += 1

    def op(self, q, fn, reads=(), writes=()):
        phys = self.ISSUER[q]
        deps = set()
        for k in reads:
            if k in self.lastw:
                deps.add(self.lastw[k])
        for k in writes:
            if k in self.lastw:
                deps.add(self.lastw[k])
            for r in self.readers.get(k, ()):
                deps.add(r)
        for (dsk, didx) in deps:
            if dsk == 'pe' and q == 'pe':
                continue
            self._need(phys, dsk, didx)
        if q in self.DMAQ:
            sk = (q, self.ndma[q] % self.NSLOT)
            self.ndma[q] += 1
            self._need(phys, sk, self.count[sk])
        else:
            sk = q
        self.count[sk] += 1
        idx = self.count[sk]
        self.streams[phys].append(('op', fn, self.sems[sk], self._inc(sk)))
        me = (sk, idx)
        for k in reads:
            self.readers.setdefault(k, []).append(me)
        for k in writes:
            self.lastw[k] = me
            self.readers[k] = []
        self.n_ops += 1
        return me

    def dma(self, q, out, in_, reads=(), writes=(), **kw):
        return self.op(q, lambda e: e.dma_start(out=out, in_=in_, **kw), reads, writes)

    def barrier(self):
        for phys in self.PHYS:
            for sk in self._semkeys():
                if self.count[sk] > 0:
                    self._need(phys, sk, self.count[sk])
        self.lastw = {}
        self.readers = {}
        if max(self.count[sk] * self._inc(sk) for sk in self._semkeys()) > 24000:
            self._new_sems()

    def finish(self, out_keys=()):
        for k in out_keys:
            if k in self.lastw:
                self._need('sp', *self.lastw[k])
        self.barrier_final()
        nc = self.nc
        streams = self.streams

        def emit(eng, items):
            for it in items:
                if it[0] == 'wait':
                    eng.wait_ge(it[1], it[2])
                else:
                    ins = it[1](eng)
                    ins.then_inc(it[2], it[3])

        with nc.Block() as block:
            @block.tensor
            def _(e):
                emit(e, streams['pe'])

            @block.scalar
            def _(e):
                emit(e, streams['act'])

            @block.vector
            def _(e):
                emit(e, streams['dve'])

            @block.gpsimd
            def _(e):
                emit(e, streams['pool'])

            @block.sync
            def _(e):
                emit(e, streams['sp'])
        self.es.close()

    def barrier_final(self):
        for sk in self._semkeys():
            if self.count[sk] > 0:
                self._need('sp', sk, self.count[sk])

    def push(self):
        if not hasattr(self, '_stack'):
            self._stack = []
        self._stack.append(self.es)
        self.es = ExitStack()

    def pop(self):
        self.barrier()
        self.es.close()
        self.es = self._stack.pop()

    def mm(self, out, lhsT, rhs, start=True, stop=True, reads=(), writes=()):
        return self.op('pe', lambda e: e.matmul(out, lhsT, rhs, start=start, stop=stop), reads, writes)

    def tr(self, out, in_, ident, reads=(), writes=()):
        return self.op('pe', lambda e: e.transpose(out, in_, ident), reads, writes)

    def act(self, out, in_, func, reads=(), writes=(), **kw):
        return self.op('act', lambda e: e.activation(out, in_, func, **kw), reads, writes)

    def tt(self, q, out, a, b, op, reads=(), writes=()):
        return self.op(q, lambda e: e.tensor_tensor(out, a, b, op), reads, writes)

    def ts(self, q, out, in0, s1, s2, op0, op1=None, reads=(), writes=(), **kw):
        if op1 is None:
            return self.op(q, lambda e: e.tensor_scalar(out, in0, s1, None, op0, **kw), reads, writes)
        return self.op(q, lambda e: e.tensor_scalar(out, in0, s1, s2, op0, op1, **kw), reads, writes)

    def stt(self, out, in0, scalar, in1, op0, op1, reads=(), writes=()):
        return self.op('dve', lambda e: e.scalar_tensor_tensor(out, in0, scalar, in1, op0, op1), reads, writes)

    def cp(self, q, out, in_, reads=(), writes=()):
        if q == 'act':
            return self.op('act', lambda e: e.copy(out, in_), reads, writes)
        return self.op(q, lambda e: e.tensor_copy(out, in_), reads, writes)

    def recip(self, out, in_, reads=(), writes=()):
        return self.op('dve', lambda e: e.reciprocal(out, in_), reads, writes)

    def memset(self, q, ap, val, writes=()):
        return self.op(q, lambda e: e.memset(ap, val), (), writes)


T = 4096
S = 2048
D = 1024
NT = 32
EPS = 1e-6
OFF = {'sh1': 0, 'sc1': 1024, 'gt1': 2048, 'sh2': 3072, 'sc2': 4096, 'gt2': 5120}


def kl(name, n):
    return [(name, i) for i in range(n)]


class K:
    pass


def setup_consts(P, k):
    nc = P.nc
    k.id32 = P.sb("id32", [128, 128], F32)
    k.idb = P.sb("idb", [128, 128], BF16)
    k.ones32 = P.sb("ones32", [128, 128], F32)
    k.onesb = P.sb("onesb", [128, 128], BF16)
    k.mU = P.sb("mU", [128, 2, 128], F32)
    k.ltri = P.sb("ltri", [128, 2, 128], F32)
    k.mUf = P.sb("mUf", [128, 128], BF16)
    P.memset('pool', k.ones32[:], 1.0, ['ones32'])
    P.memset('pool', k.onesb[:], 1.0, ['onesb'])
    P.op('pool', lambda e: e.affine_select(k.id32[:], k.ones32[:], [[-1, 128]], ALU.is_equal, 0.0, base=0, channel_multiplier=1),
         ['ones32'], ['id32'])
    P.cp('pool', k.idb[:], k.id32[:], ['id32'], ['idb'])
    tmp = P.sb("ctmp", [128, 128], F32)
    P.op('pool', lambda e: e.affine_select(tmp[:], k.ones32[:], [[1, 128]], ALU.is_ge, 0.0, base=0, channel_multiplier=-1),
         ['ones32'], ['ctmp'])
    P.cp('pool', k.mUf[:], tmp[:], ['ctmp'], ['mUf'])
    P.cp('pool', k.mU[:, 0, :], tmp[:], ['ctmp'], ['mU'])
    P.memset('pool', k.mU[0:64, 0, 64:128], 0.0, ['mU'])
    P.tt('pool', k.mU[:, 1, :], k.mU[:, 0, :], k.id32[:], ALU.subtract, ['mU', 'id32'], ['mU'])
    P.cp('pool', k.ltri[:, 0, :], k.mU[:, 0, :], ['mU'], ['ltri'])
    P.memset('pool', k.ltri[:, 1, :], 0.0, ['ltri'])
    P.memset('pool', k.ltri[0:64, 1, 0:64], 1.0, ['ltri'])
    P.memset('pool', k.ltri[64:128, 1, 64:128], 1.0, ['ltri'])


def phase_mod(P, k):
    P.push()
    cT = P.sb("cT", [128, 8, 2], F32)
    scT = P.sb("scT", [128, 8, 2], F32)
    ab = P.sb("ab", [2, 2, 6144], F32)
    msb = P.sb("msb", [2, 2, 6144], F32)
    wa = [P.sb("wa%d" % i, [128, 8, 512], F32) for i in range(2)]
    psm = [P.ps("psm%d" % i, [128, 512]) for i in range(2)]
    for b in range(2):
        P.dma('sp', cT[:, :, b], k.c[b, :].rearrange("(kc p) -> p kc", p=128), [], ['cT'], allow_slow_non_contiguous=True)
    P.act(scT[:], cT[:], AF.Silu, ['cT'], ['scT'])
    for l in range(2):
        P.dma('sp', ab[:, l, :], k.ada_b[l:l + 1, :].partition_broadcast(2), [], ['ab'])
    i = 0
    for l in range(2):
        for cb in range(12):
            j = i % 2
            P.dma('sp' if j == 0 else 'actq', wa[j][:], k.ada_w[l, :, cb * 512:(cb + 1) * 512].rearrange("(kc p) n -> p kc n", p=128),
                  [], ['wa%d' % j])
            for kc in range(8):
                P.mm(psm[j][0:2, :], scT[:, kc, :], wa[j][:, kc, :], kc == 0, kc == 7, ['scT', 'wa%d' % j], ['psm%d' % j])
            P.tt('dve', msb[:, l, cb * 512:(cb + 1) * 512], psm[j][0:2, :], ab[:, l, cb * 512:(cb + 1) * 512], ALU.add,
                 ['psm%d' % j, 'ab'], ['msb'])
            i += 1
    P.dma('sp', k.modrows.rearrange("l b n -> b l n"), msb[:], ['msb'], ['modrows'])
    P.pop()


class Normer:
    def __init__(self, P, k, l, which, src, f32):
        self.P, self.k, self.src, self.f32 = P, k, src, f32
        g_ap = (k.norm1_g if which == 1 else k.norm2_g)
        gcol = P.sb("n_gcol", [128, 8], F32)
        P.dma('sp', gcol[:], g_ap[l, :].rearrange("(kc p) -> p kc", p=128), [], ['n_gcol'], allow_slow_non_contiguous=True)
        self.A, self.B = [], []
        for b in range(2):
            sc = P.sb("n_sc%d" % b, [128, 8], F32)
            A = P.sb("n_A%d" % b, [128, 8], F32)
            B = P.sb("n_B%d" % b, [128, 8], F32)
            o = OFF['sc%d' % which]
            P.dma('sp', sc[:], k.modrows[l, b, o:o + 1024].rearrange("(kc p) -> p kc", p=128), [], ['n_sc%d' % b],
                  allow_slow_non_contiguous=True)
            o = OFF['sh%d' % which]
            P.dma('sp', B[:], k.modrows[l, b, o:o + 1024].rearrange("(kc p) -> p kc", p=128), [], ['n_B%d' % b],
                  allow_slow_non_contiguous=True)
            P.stt(A[:], sc[:], 1.0, gcol[:], ALU.add, ALU.mult, ['n_sc%d' % b, 'n_gcol'], ['n_A%d' % b])
            self.A.append(A)
            self.B.append(B)
        dt = F32 if f32 else BF16
        self.xt = [P.sb("n_xt%d" % i, [128, 1024], F32) for i in range(2)]
        self.xn = [P.sb("n_xn%d" % i, [128, 1024], dt) for i in range(2)]
        self.junk = P.sb("n_junk", [128, 1024], BF16)
        self.st = P.sb("n_st", [128, 2, 4], F32)
        self.pst = [P.ps("n_pst%d" % i, [128, 8, 128], dt) for i in range(2)]
        self.ident = k.id32 if f32 else k.idb
        self.i = 0

    def tile(self, ti, out_fn, out_keys):
        self.tile_a(ti)
        self.tile_b(ti, out_fn, out_keys)

    def tile_a(self, ti):
        P, k = self.P, self.k
        j = self.i % 2
        self.i += 1
        self.slot = getattr(self, 'slot', {})
        self.slot[ti] = j
        xt, xn, st = self.xt[j], self.xn[j], self.st
        kx, kn, ks = 'n_xt%d' % j, 'n_xn%d' % j, 'n_st%d' % j
        P.dma('sp', xt[:], self.src[ti * 128:(ti + 1) * 128, :], ['xsrc%d' % ti], [kx])
        P.act(self.junk[:], xt[:], AF.Square, [kx], ['n_junk', ks], accum_out=st[:, j, 0:1])
        P.act(st[:, j, 1:2], st[:, j, 0:1], AF.Sqrt, [ks], [ks], bias=EPS, scale=1.0 / 1024)
        P.recip(st[:, j, 2:3], st[:, j, 1:2], [ks], [ks])
        if self.f32:
            P.act(xn[:], xt[:], AF.Copy, [kx, ks], [kn], scale=st[:, j, 2:3])
        else:
            P.ts('dve', xn[:], xt[:], st[:, j, 2:3], None, ALU.mult, None, [kx, ks], [kn])

    def tile_b(self, ti, out_fn, out_keys):
        P, k = self.P, self.k
        j = self.slot[ti]
        b = ti // 16
        xn, pst = self.xn[j], self.pst[j]
        kn, kp = 'n_xn%d' % j, 'n_pst%d' % j
        for kc in range(8):
            P.tr(pst[:, kc, :], xn[:, kc * 128:(kc + 1) * 128], self.ident[:], [kn, 'idb', 'id32'], [kp])
        for kc in range(8):
            dst = out_fn(kc)
            if j == 0:
                P.act(dst, pst[:, kc, :], AF.Identity, [kp, 'n_A%d' % b, 'n_B%d' % b], [out_keys[kc]],
                      bias=self.B[b][:, kc:kc + 1], scale=self.A[b][:, kc:kc + 1])
            else:
                P.ts('dve', dst, pst[:, kc, :], self.A[b][:, kc:kc + 1], self.B[b][:, kc:kc + 1], ALU.mult, ALU.add,
                     [kp, 'n_A%d' % b, 'n_B%d' % b], [out_keys[kc]])


def load_w_bf16(P, dst, src, key, nsplit=8):
    kcs = dst.shape[1]
    for kc in range(kcs):
        P.dma('poolq', dst[:, kc, :], src[kc * 128:(kc + 1) * 128, :], [], [key])


def ftile_col(f):
    return f * 128 if f < 16 else 2056 + (f - 16) * 128


def phase_e1(P, k, xsrc):
    P.push()
    W = P.sb("e1_W", [128, 8, 3596], BF16)
    load_w_bf16(P, W, k.ev_w_in[0], 'e1_W')
    nm = Normer(P, k, 0, 1, xsrc, False)
    hT = [P.sb("e1_hT%d" % i, [128, 8, 512], BF16) for i in range(2)]
    psf = [P.ps("e1_psf%d" % i, [128, 512]) for i in range(3)]
    psv = P.ps("e1_psv", [128, 512])
    psab = P.ps("e1_psab", [128, 4, 8])
    psff = P.ps("e1_psff", [128, 512])
    stg = [P.sb("e1_stg%d" % i, [128, 512], BF16) for i in range(4)]
    stv = [P.sb("e1_stv%d" % i, [128, 512], BF16) for i in range(2)]
    stab = [P.sb("e1_stab%d" % i, [128, 4, 8], F32) for i in range(2)]
    ff = P.sb("e1_ff", [4, T], F32)
    fbc = P.sb("e1_fbc", [4, 2], F32)
    P.dma('sp', fbc[:, 0:1], k.ev_fox_f_bias.rearrange("o h -> h o"), [], ['e1_fbc'], allow_slow_non_contiguous=True)
    P.ts('dve', fbc[:, 1:2], fbc[:, 0:1], -1.0, None, ALU.mult, None, ['e1_fbc'], ['e1_fbc'])
    nst = [0]

    def norm_a(tb, j):
        nm.tile_a(tb * 4 + j)

    def norm_b(tb, j):
        h = hT[tb % 2]
        hk = [('e1_hT%d' % (tb % 2), kc) for kc in range(8)]
        nm.tile_b(tb * 4 + j, lambda kc, h=h, j=j: h[:, kc, j * 128:(j + 1) * 128], hk)

    def norm_tile(tb, j):
        norm_a(tb, j)
        norm_b(tb, j)

    def fillers(tb):
        fs = [lambda: norm_a(tb, 0)]
        for j in range(1, 4):
            fs.append(lambda j=j: (norm_b(tb, j - 1), norm_a(tb, j)))
        fs.append(lambda: norm_b(tb, 3))
        return fs

    def units(tb):
        h = hT[tb % 2]
        hk = [('e1_hT%d' % (tb % 2), kc) for kc in range(8)]
        us = []

        def ftile(f):
            ib = f % 3
            ps = psf[ib]
            c0 = ftile_col(f)
            for kc in range(8):
                P.mm(ps[:], W[:, kc, c0:c0 + 128], h[:, kc, :], kc == 0, kc == 7, ['e1_W'] + hk, ['e1_psf%d' % ib])
            sg = stg[nst[0] % 4]
            sk = 'e1_stg%d' % (nst[0] % 4)
            nst[0] += 1
            P.cp('act' if ib != 1 else 'dve', sg[:], ps[:], ['e1_psf%d' % ib], [sk])
            P.dma('sp', k.projT[f * 128:(f + 1) * 128, tb * 512:(tb + 1) * 512], sg[:], [sk], [('projT', f, tb)])

        def fvtile(j):
            for kc in range(8):
                P.mm(psv[:], h[:, kc, j * 128:(j + 1) * 128], W[:, kc, 3080:3592], kc == 0, kc == 7, ['e1_W'] + hk, ['e1_psv'])
            sv = stv[j % 2]
            P.cp('act', sv[:], psv[:], ['e1_psv'], ['e1_stv%d' % (j % 2)])
            t0 = tb * 512 + j * 128
            P.dma('sp', k.fvtm[t0:t0 + 128, :], sv[:], ['e1_stv%d' % (j % 2)], [('fvtm', tb, j)])
            for kc in range(8):
                P.mm(psab[:, j, :], h[:, kc, j * 128:(j + 1) * 128], W[:, kc, 2048:2056], kc == 0, kc == 7, ['e1_W'] + hk, ['e1_psab'])

        def tail():
            sa = stab[tb % 2]
            P.cp('dve', sa[:], psab[:], ['e1_psab'], ['e1_stab%d' % (tb % 2)])
            P.dma('sp', k.abtm[tb * 512:(tb + 1) * 512, :].rearrange("(j p) c -> p j c", p=128), sa[:], ['e1_stab%d' % (tb % 2)],
                  [('abtm', tb)])
            for kc in range(8):
                P.mm(psff[0:4, :], W[:, kc, 3592:3596], h[:, kc, :], kc == 0, kc == 7, ['e1_W'] + hk, ['e1_psff'])
            P.cp('dve', ff[:, tb * 512:(tb + 1) * 512], psff[0:4, :], ['e1_psff'], ['e1_ff'])

        for f in range(24):
            us.append(lambda f=f: ftile(f))
        for j in range(4):
            us.append(lambda j=j: fvtile(j))
        us.append(tail)
        return us

    for j in range(4):
        norm_tile(0, j)
    for tb in range(8):
        us = units(tb)
        fill = fillers(tb + 1) if tb + 1 < 8 else []
        for i, u_ in enumerate(us):
            u_()
            if fill and i % 6 == 4:
                fill.pop(0)()
        while fill:
            fill.pop(0)()
    P.dma('sp', k.ffrows, ff[:], ['e1_ff'], ['ffrows'])
    P.pop()


def phase_e1b(P, k):
    P.push()
    ff = P.sb("e1b_ff", [4, T], F32)
    fbc = P.sb("e1b_fbc", [4, 2], F32)
    P.dma('sp', ff[:], k.ffrows, [], ['e1b_ff'])
    P.dma('sp', fbc[:, 0:1], k.ev_fox_f_bias.rearrange("o h -> h o"), [], ['e1b_fbc'], allow_slow_non_contiguous=True)
    P.ts('dve', fbc[:, 1:2], fbc[:, 0:1], -1.0, None, ALU.mult, None, ['e1b_fbc'], ['e1b_fbc'])
    lf = P.sb("e1b_lf", [4, T], F32)
    cum = P.sb("e1b_cum", [4, 2, T], F32)
    one4 = P.sb("e1b_one4", [4, S], F32)
    P.memset('pool', one4[:], 1.0, ['e1b_one4'])
    P.act(lf[:], ff[:], AF.Exp, ['e1b_ff', 'e1b_fbc'], ['e1b_lf'], bias=fbc[:, 1:2], scale=-1.0)
    P.act(lf[:], lf[:], AF.Ln, ['e1b_lf'], ['e1b_lf'], bias=1.0, scale=1.0)
    P.ts('dve', lf[:], lf[:], -1.0, None, ALU.mult, None, ['e1b_lf'], ['e1b_lf'])
    for s in range(2):
        P.op('dve', lambda e, s=s: e.tensor_tensor_scan(cum[:, 0, s * S:(s + 1) * S], one4[:], lf[:, s * S:(s + 1) * S], 0.0,
                                                         ALU.mult, ALU.add), ['e1b_lf', 'e1b_one4'], ['e1b_cum'])
    P.ts('dve', cum[:, 1, :], cum[:, 0, :], -1.0, None, ALU.mult, None, ['e1b_cum'], ['e1b_cum'])
    P.dma('sp', k.cumrows.rearrange("(a h) t -> h a t", a=2), cum[:], ['e1b_cum'], ['cumrows'])
    hl = P.sb("e1b_hl", [4, 4, T], BF16)
    hif = P.sb("e1b_hif", [4, T], F32)
    for a in range(2):
        kd = 2 * (1 - a)
        P.cp('act', hl[:, kd, :], cum[:, a, :], ['e1b_cum'], ['e1b_hl'])
        P.cp('act', hif[:], hl[:, kd, :], ['e1b_hl'], ['e1b_hif'])
        P.tt('dve', hl[:, kd + 1, :], cum[:, a, :], hif[:], ALU.subtract, ['e1b_cum', 'e1b_hif'], ['e1b_hl'])
    P.dma('sp', k.cumhl, hl[:], ['e1b_hl'], ['cumhl'])
    P.pop()


IN_SPECS = [("x", [T, D]), ("c", [2, D]), ("ada_w", [2, 1024, 6144]), ("ada_b", [2, 6144]), ("norm1_g", [2, 1024]),
            ("norm2_g", [2, 1024]), ("ev_w_in", [1, 1024, 3596]), ("ev_conv_w", [1, 4, 1536]), ("ev_dn_a_log", [1, 4]),
            ("ev_dn_dt_bias", [1, 4]), ("ev_dn_onorm_g", [1, 128]), ("ev_fox_f_bias", [1, 4]), ("ev_fox_qnorm_g", [1, 128]),
            ("ev_fox_knorm_g", [1, 128]), ("ev_w_out", [1, 1024, 1024]), ("od_w_in", [1, 1024, 2048]),
            ("od_conv_w", [1, 4, 512]), ("od_conv_b", [1, 512]), ("od_lru_wr", [1, 4, 128, 128]), ("od_lru_br", [1, 512]),
            ("od_lru_wi", [1, 4, 128, 128]), ("od_lru_bi", [1, 512]), ("od_lru_lambda", [1, 512]),
            ("od_sgu_norm_g", [1, 512]), ("od_sgu_w", [1, 4, 128, 128]), ("od_sgu_b", [1, 4, 128]),
            ("od_w_out", [1, 1024, 1024]), ("router_w", [1024, 16]), ("router_b", [16]),
            ("moe_w_gate", [2, 16, 1024, 256]), ("moe_w_up", [2, 16, 1024, 256]), ("moe_w_down", [2, 16, 256, 1024])]

SCRATCH = [("modrows", [2, 2, 6144], F32), ("projT", [3072, T], BF16), ("fvtm", [T, 512], BF16), ("abtm", [T, 8], F32),
           ("cumrows", [8, T], F32), ("ffrows", [4, T], F32), ("cumhl", [4, 4, T], BF16), ("dnT", [1536, T], BF16), ("omixT", [1024, T], BF16),
           ("xa", [T, D], F32), ("xb", [T, D], F32), ("proj1T", [1536, T], BF16), ("svtm", [T, 512], BF16)]


def build(dbg=(), stop=None):
    nc = bass.Bass("TRN2", target_bir_lowering=False)
    k = K()
    for name, shape in IN_SPECS:
        setattr(k, name, nc.dram_tensor(name, list(shape), F32, kind="ExternalInput").ap())
    for name, shape, dt in SCRATCH:
        kind = "ExternalOutput" if name in dbg else "Internal"
        setattr(k, name, nc.dram_tensor(name, list(shape), dt, kind=kind).ap())
    k.out = nc.dram_tensor("out", [T, D], F32, kind="ExternalOutput").ap()
    if "dbg_gates" in dbg:
        k.dbg_gates = nc.dram_tensor("dbg_gates", [2, 128, 16, 16], F32, kind="ExternalOutput").ap()
    P = Prog(nc)
    setup_consts(P, k)
    phases = [
        ('mod', lambda: phase_mod(P, k)),
        ('e1', lambda: (phase_e1(P, k, k.x), phase_e1b(P, k))),
        ('e3', lambda: phase_e3(P, k)),
        ('e2a', lambda: phase_e2a(P, k)),
        ('e2b', lambda: phase_e2b(P, k)),
        ('e4', lambda: phase_outproj(P, k, k.ev_w_out[0], 0, k.x, k.xa, 'e4')),
        ('m0', lambda: phase_moe(P, k, 0, k.xa, k.xb, 'm0')),
        ('o1', lambda: phase_o1(P, k, k.xb)),
        ('o2', lambda: phase_o2(P, k)),
        ('o3', lambda: phase_o3(P, k)),
        ('o4', lambda: phase_outproj(P, k, k.od_w_out[0], 1, k.xb, k.xa, 'o4')),
        ('m1', lambda: phase_moe(P, k, 1, k.xa, k.out, 'm1')),
    ]
    for name, fn in phases:
        fn()
        if stop == name:
            break
    P.finish()
    return nc, P


def make_in_map(inputs, core):
    m = {}
    for name, shape in IN_SPECS:
        a = inputs[name]
        if name == "x":
            a = a[2 * core:2 * core + 2].reshape(T, D)
        elif name == "c":
            a = a[2 * core:2 * core + 2]
        m[name] = np.ascontiguousarray(a, dtype=np.float32)
    return m


def phase_e3(P, k):
    P.push()
    gq = P.sb("e3_gq", [128, 2], F32)
    gs = P.sb("e3_gs", [128, 2], F32)
    negm = P.sb("e3_negm", [128, 1], F32)
    mrow = P.sb("e3_mrow", [1, 4], F32)
    psx = P.ps("e3_psx", [128, 512])
    P.dma('sp', gq[:, 0:1], k.ev_fox_qnorm_g.rearrange("o d -> d o"), [], ['e3_gq'], allow_slow_non_contiguous=True)
    P.dma('sp', gq[:, 1:2], k.ev_fox_knorm_g.rearrange("o d -> d o"), [], ['e3_gq'], allow_slow_non_contiguous=True)
    P.ts('dve', gs[:, 0:1], gq[:, 0:1], 128 ** -0.5, None, ALU.mult, None, ['e3_gq'], ['e3_gs'])
    P.cp('dve', gs[:, 1:2], gq[:, 1:2], ['e3_gq'], ['e3_gs'])
    for i in range(2):
        P.mm(psx[0:1, i * 128:(i + 1) * 128], gq[:, i:i + 1], k.id32[:], True, True, ['e3_gq', 'id32'], ['e3_psx'])
        P.op('dve', lambda e, i=i: e.tensor_reduce(mrow[:, i:i + 1], psx[0:1, i * 128:(i + 1) * 128], AX.X, ALU.max,
                                                   apply_absolute_value=True), ['e3_psx'], ['e3_mrow'])
    P.tt('dve', mrow[:, 2:3], mrow[:, 0:1], mrow[:, 1:2], ALU.mult, ['e3_mrow'], ['e3_mrow'])
    P.mm(psx[:, 256:257], k.ones32[0:1, :], mrow[:, 2:3], True, True, ['e3_mrow', 'ones32'], ['e3_psx'])
    P.ts('dve', negm[:], psx[:, 256:257], -(128 ** 0.5) * 1.02, None, ALU.mult, None, ['e3_psx'], ['e3_negm'])

    raw = [[P.sb("e3_raw%d_%d" % (p_, i), [128, S], BF16) for i in range(2)] for p_ in range(2)]
    sq = [P.sb("e3_sq%d" % i, [128, 512], BF16) for i in range(2)]
    rt = [P.sb("e3_rt%d" % i, [128, 512], F32) for i in range(2)]
    qn = [P.sb("e3_qn%d" % i, [128, 512], F32) for i in range(2)]
    qk = [[P.sb("e3_qk%d_%d" % (p_, i), [128, S], BF16) for i in range(2)] for p_ in range(2)]
    V = [P.sb("e3_V%d" % p_, [128, 16, 128], BF16) for p_ in range(2)]
    CK = [P.sb("e3_CK%d" % p_, [4, S], BF16) for p_ in range(2)]
    CQ = [P.sb("e3_CQ%d" % p_, [4, S], BF16) for p_ in range(2)]
    for p_ in range(2):
        P.memset('pool', CK[p_][:], 1.0, ['e3_CK%d' % p_])
        P.memset('pool', CQ[p_][:], 1.0, ['e3_CQ%d' % p_])
    psn = P.ps("e3_psn", [128, 512])
    pss = [P.ps("e3_pss%d" % i, [128, 512]) for i in range(2)]
    pso = [P.ps("e3_pso%d" % i, [128, 512]) for i in range(2)]
    psd = [P.ps("e3_psd%d" % i, [128, 512]) for i in range(2)]
    ptb = [P.sb("e3_pt%d" % i, [128, 512], BF16) for i in range(3)]
    rden = P.sb("e3_rden", [128, 512], F32)
    ot = [P.sb("e3_ot%d" % i, [128, 512], BF16) for i in range(2)]
    heads = [(s_, h) for s_ in range(2) for h in range(4)]

    def loads(n):
        s_, h = heads[n]
        p_ = n % 2
        t0 = s_ * S
        for i in range(2):
            r0 = 2048 + i * 512 + h * 128
            P.dma('actq', raw[p_][i][:], k.projT[r0:r0 + 128, t0:t0 + S], [], ['e3_raw%d_%d' % (p_, i)])
        P.dma('actq', V[p_][:], k.fvtm[t0:t0 + S, h * 128:(h + 1) * 128].rearrange("(j p) d -> p j d", p=128), [], ['e3_V%d' % p_])
        P.dma('actq', CK[p_][0:2, :], k.cumhl[h, 0:2, t0:t0 + S], [], ['e3_CK%d' % p_])
        P.dma('actq', CQ[p_][2:4, :], k.cumhl[h, 2:4, t0:t0 + S], [], ['e3_CQ%d' % p_])

    nbk = [0]

    def norm_block(n, i, b):
        p_ = n % 2
        j = nbk[0] % 2
        nbk[0] += 1
        cs = slice(b * 512, (b + 1) * 512)
        kr = 'e3_raw%d_%d' % (p_, i)
        P.tt('dve', sq[j][:], raw[p_][i][:, cs], raw[p_][i][:, cs], ALU.mult, [kr], ['e3_sq%d' % j])
        pz = psx if j == 0 else psn
        kz = 'e3_psx' if j == 0 else 'e3_psn'
        P.mm(pz[:], k.onesb[:], sq[j][:], True, True, ['e3_sq%d' % j, 'onesb'], [kz])
        P.act(rt[j][:], pz[:], AF.Ln, [kz], ['e3_rt%d' % j], bias=EPS, scale=1.0 / 128)
        P.act(rt[j][:], rt[j][:], AF.Exp, ['e3_rt%d' % j], ['e3_rt%d' % j], scale=-0.5)
        P.tt('dve', qn[j][:], raw[p_][i][:, cs], rt[j][:], ALU.mult, [kr, 'e3_rt%d' % j], ['e3_qn%d' % j])
        P.act(qk[p_][i][:, cs], qn[j][:], AF.Copy, ['e3_qn%d' % j, 'e3_gs'], [('e3_qk%d_%d' % (p_, i), b)], scale=gs[:, i:i + 1])

    cnt = {'it': 0, 'nq': 0}

    def attention(n, filler):
        s_, h = heads[n]
        p_ = n % 2
        t0 = s_ * S
        qT, kT = qk[p_]
        kq = [('e3_qk%d_%d' % (p_, i), b) for i in range(2) for b in range(4)]
        its = []
        for Q in range(4):
            for j in range(4 * Q + 4):
                its.append((Q, j))

        def scores(m):
            Q, j = its[m]
            r = j - 4 * Q
            c0 = max(r, 0) * 128
            i2 = (cnt['it'] + m) % 2
            ps_s = pss[i2]
            q0 = Q * 512 + c0
            P.mm(ps_s[:, c0:512], kT[:, j * 128:(j + 1) * 128], qT[:, q0:(Q + 1) * 512], True, False, kq, ['e3_pss%d' % i2])
            P.mm(ps_s[:, c0:512], CK[p_][:, j * 128:(j + 1) * 128], CQ[p_][:, q0:(Q + 1) * 512], False, True,
                 ['e3_CK%d' % p_, 'e3_CQ%d' % p_], ['e3_pss%d' % i2])

        scores(0)
        for m in range(len(its)):
            Q, j = its[m]
            last = 4 * Q + 3
            r = j - 4 * Q
            c0 = max(r, 0) * 128
            i2 = (cnt['it'] + m) % 2
            i3 = (cnt['it'] + m) % 3
            ps_s, pT = pss[i2], ptb[i3]
            ks, kp = 'e3_pss%d' % i2, 'e3_pt%d' % i3
            nq = cnt['nq']
            po, pd = pso[nq % 2], psd[nq % 2]
            ko, kd = 'e3_pso%d' % (nq % 2), 'e3_psd%d' % (nq % 2)
            P.act(pT[:, c0:512], ps_s[:, c0:512], AF.Exp, [ks, 'e3_negm'], [kp], bias=negm[:, 0:1])
            if r >= 0:
                P.tt('dve', pT[:, c0:c0 + 128], pT[:, c0:c0 + 128], k.mUf[:], ALU.mult, [kp, 'mUf'], [kp])
            if m + 1 < len(its):
                scores(m + 1)
            P.mm(po[:, c0:512], V[p_][:, j, :], pT[:, c0:512], j == 0, j == last, ['e3_V%d' % p_, kp], [ko])
            P.mm(pd[:, c0:512], k.onesb[:], pT[:, c0:512], j == 0, j == last, ['onesb', kp], [kd])
            if j == last:
                o = ot[nq % 2]
                P.act(rden[:], pd[:], AF.Ln, [kd], ['e3_rden'])
                P.act(rden[:], rden[:], AF.Exp, ['e3_rden'], ['e3_rden'], scale=-1.0)
                P.tt('dve', o[:], po[:], rden[:], ALU.mult, [ko, 'e3_rden'], ['e3_ot%d' % (nq % 2)])
                P.dma('sp', k.omixT[512 + h * 128:512 + (h + 1) * 128, t0 + Q * 512:t0 + (Q + 1) * 512], o[:],
                      ['e3_ot%d' % (nq % 2)], [('omixT', h, s_, Q)])
                cnt['nq'] += 1
            if m % 5 == 4 and filler:
                filler.pop(0)()
        while filler:
            filler.pop(0)()
        cnt['it'] += len(its)

    loads(0)
    for i in range(2):
        for b in range(4):
            norm_block(0, i, b)
    for n in range(8):
        filler = []
        if n + 1 < 8:
            loads(n + 1)
            filler = [(lambda i=i, b=b, n=n: norm_block(n + 1, i, b)) for i in range(2) for b in range(4)]
        attention(n, filler)
    P.pop()


def phase_e2a(P, k):
    P.push()
    cw = P.sb("e2a_cw", [128, 12, 4], F32)
    for kk in range(4):
        P.dma('sp', cw[:, :, kk], k.ev_conv_w[0, kk, :].rearrange("(ti p) -> p ti", p=128), [], ['e2a_cw'],
              allow_slow_non_contiguous=True)
    dg = P.sb("e2a_dg", [128, 12, 4, 128], BF16)
    for ti in range(12):
        for kk in range(4):
            P.ts('dve', dg[:, ti, kk, :], k.id32[:], cw[:, ti, kk:kk + 1], None, ALU.mult, None, ['e2a_cw', 'id32'], ['e2a_dg'])
    raw = [P.sb("e2a_raw%d" % i, [128, 3 + S], BF16) for i in range(2)]
    for i in range(2):
        P.memset('pool', raw[i][:, 0:3], 0.0, ['e2a_raw%d' % i])
    psc = [P.ps("e2a_psc%d" % i, [128, 512]) for i in range(2)]
    psn2 = [P.ps("e2a_psn%d" % i, [128, 512]) for i in range(2)]
    cs = P.sb("e2a_cs", [128, S], F32)
    sq2 = [P.sb("e2a_sq%d" % i, [128, 512], BF16) for i in range(2)]
    rt2 = [P.sb("e2a_rt%d" % i, [128, 512], F32) for i in range(2)]
    ob = [P.sb("e2a_ob%d" % i, [128, S], BF16) for i in range(2)]
    n = 0
    nb = 0
    for s in range(2):
        t0 = s * S
        for ti in range(12):
            rw = raw[n % 2]
            kr = 'e2a_raw%d' % (n % 2)
            o = ob[n % 2]
            ko = 'e2a_ob%d' % (n % 2)
            n += 1
            P.dma('sp', rw[:, 3:3 + S], k.projT[ti * 128:(ti + 1) * 128, t0:t0 + S], [], [kr])
            for b in range(4):
                pc = psc[nb % 2]
                kc_ = 'e2a_psc%d' % (nb % 2)
                nb += 1
                for kk in range(4):
                    P.mm(pc[:], dg[:, ti, kk, :], rw[:, b * 512 + kk:b * 512 + kk + 512], kk == 0, kk == 3, ['e2a_dg', kr], [kc_])
                if ti < 8:
                    P.act(cs[:, b * 512:(b + 1) * 512], pc[:], AF.Silu, [kc_], [('e2a_cs', b)])
                else:
                    P.act(o[:, b * 512:(b + 1) * 512], pc[:], AF.Silu, [kc_], [ko])
            if ti < 8:
                sc = 128 ** -0.5 if ti < 4 else 1.0
                for b in range(4):
                    c_ = slice(b * 512, (b + 1) * 512)
                    i2 = b % 2
                    sq, rt, psn = sq2[i2], rt2[i2], psn2[i2]
                    ksq, krt, kpn = 'e2a_sq%d' % i2, 'e2a_rt%d' % i2, 'e2a_psn%d' % i2
                    P.tt('dve', sq[:], cs[:, c_], cs[:, c_], ALU.mult, [('e2a_cs', b)], [ksq])
                    P.mm(psn[:], k.onesb[:], sq[:], True, True, [ksq, 'onesb'], [kpn])
                    P.act(rt[:], psn[:], AF.Ln, [kpn], [krt], bias=EPS, scale=1.0)
                    P.act(rt[:], rt[:], AF.Exp, [krt], [krt], scale=-0.5)
                    P.stt(o[:, c_], cs[:, c_], sc, rt[:], ALU.mult, ALU.mult, [('e2a_cs', b), krt], [ko])
            P.dma('sp', k.dnT[ti * 128:(ti + 1) * 128, t0:t0 + S], o[:], [ko], [('dnT', ti, s)])
    P.pop()


def phase_e2b(P, k):
    P.push()
    X = [P.ps("e2_X%d" % g, [128, 512]) for g in range(4)]
    Y = [P.ps("e2_Y%d" % g, [128, 512]) for g in range(4)]
    wT = P.sb("e2_wT", [128, 4, S], BF16)
    uu = P.sb("e2_u", [128, 4, 16, 128], BF16)
    qdT = P.sb("e2_qdT", [128, 4, S], BF16)
    qkT = P.sb("e2_qkT", [128, 4, 16, 128], BF16)
    kdec = P.sb("e2_kdec", [128, 4, 16, 128], BF16)
    glast = P.sb("e2_glast", [128, 4, 32], F32)
    oT = P.sb("e2_oT", [128, 4, S], F32)
    qin = [P.sb("e2_qin%d" % i, [128, 3, S], BF16) for i in range(2)]
    ab = P.sb("e2_ab", [128, 16, 8], F32)
    cst = P.sb("e2_cst", [128, 3, 4], F32)
    gw = P.sb("e2_gw", [128, 8, 64], F32)
    sp = P.sb("e2_sp", [128, 16, 4], F32)
    ocol = P.sb("e2_ocol", [128, 1], F32)
    P.dma('sp', cst[:, 0, :], k.ev_dn_dt_bias[0:1, :].partition_broadcast(128), [], ['e2_cst'])
    P.dma('sp', cst[:, 1, :], k.ev_dn_a_log[0:1, :].partition_broadcast(128), [], ['e2_cst'])
    P.dma('sp', ocol[:], k.ev_dn_onorm_g.rearrange("o d -> d o"), [], ['e2_ocol'], allow_slow_non_contiguous=True)
    P.act(cst[:, 2, :], cst[:, 1, :], AF.Exp, ['e2_cst'], ['e2_cst'])
    P.ts('dve', cst[:, 2, :], cst[:, 2, :], -1.0, None, ALU.mult, None, ['e2_cst'], ['e2_cst'])
    rhsd = [P.sb("e2_rhsd%d" % g, [128, 2, 128], F32) for g in range(4)]
    t1 = [P.sb("e2_t1%d" % g, [128, 256], F32) for g in range(4)]
    EE = [P.sb("e2_EE%d" % g, [128, 256], F32) for g in range(4)]
    egc = [P.sb("e2_egc%d" % g, [128, 128], F32) for g in range(4)]
    prod = [P.sb("e2_prod%d" % g, [128, 256], F32) for g in range(4)]
    pp = [[P.sb("e2_pp%d_%d" % (g, i), [128, 256], BF16) for i in range(2)] for g in range(4)]
    RR = [[P.sb("e2_R%d_%d" % (g, i), [128, 128], BF16) for i in range(2)] for g in range(4)]
    TTb = [P.sb("e2_TTb%d" % g, [128, 128], BF16) for g in range(4)]
    kbg = [P.sb("e2_kbg%d" % g, [128, 128], BF16) for g in range(4)]
    vb = [P.sb("e2_vb%d" % g, [128, 128], BF16) for g in range(4)]
    S32 = [P.sb("e2_S32_%d" % g, [128, 128], F32) for g in range(4)]
    Sb = [P.sb("e2_Sb%d" % g, [128, 128], BF16) for g in range(4)]
    vnb = [P.sb("e2_vnb%d" % g, [128, 128], BF16) for g in range(4)]
    zt = P.sb("e2_zt", [128, S], BF16)
    sq = P.sb("e2_sq", [128, 512], BF16)
    rt = P.sb("e2_rt", [128, 512], F32)
    on = P.sb("e2_on", [128, 512], F32)
    sz = P.sb("e2_sz", [128, S], BF16)
    fin = [P.sb("e2_fin%d" % i, [128, 512], BF16) for i in range(2)]
    nin = 0
    nfin = 0
    for s in range(2):
        t0 = s * S
        P.dma('sp', ab[:], k.abtm[t0:t0 + S, :].rearrange("(j p) c -> p j c", p=128), [], ['e2_ab'])
        for h in range(4):
            P.act(sp[:, :, h], ab[:, :, h], AF.Exp, ['e2_ab', 'e2_cst'], ['e2_sp'], bias=cst[:, 0, h:h + 1])
        P.act(sp[:], sp[:], AF.Ln, ['e2_sp'], ['e2_sp'], bias=1.0)
        ld = gw[:, 0, :].rearrange("p (j h) -> p j h", h=4)
        for h in range(4):
            P.ts('dve', ld[:, :, h], sp[:, :, h], cst[:, 2, h:h + 1], None, ALU.mult, None, ['e2_sp', 'e2_cst'], ['e2_gw0'])
        P.act(gw[:, 1, :].rearrange("p (j h) -> p j h", h=4), ab[:, :, 4:8], AF.Sigmoid, ['e2_ab'], ['e2_gw1'])
        P.act(gw[:, 2, :], gw[:, 1, :], AF.Ln, ['e2_gw1'], ['e2_gw2'])
        P.mm(Y[0][:, 0:64], k.ltri[:, 0, :], gw[:, 0, :], True, True, ['ltri', 'e2_gw0'], ['e2_Y0a'])
        P.mm(Y[0][:, 64:128], k.ltri[:, 1, :], gw[:, 0, :], True, True, ['ltri', 'e2_gw0'], ['e2_Y0a'])
        P.cp('dve', gw[:, 3:5, :], Y[0][:, 0:128].rearrange("p (a n) -> p a n", a=2), ['e2_Y0a'], ['e2_gw3', 'e2_gw4'])
        P.act(gw[:, 5, :], gw[:, 3, :], AF.Exp, ['e2_gw3'], ['e2_gw5'])
        P.tt('dve', gw[:, 5, :], gw[:, 5, :], gw[:, 1, :], ALU.mult, ['e2_gw5', 'e2_gw1'], ['e2_gw5'])
        P.tt('dve', gw[:, 6, :], gw[:, 4, :], gw[:, 3, :], ALU.subtract, ['e2_gw3', 'e2_gw4'], ['e2_gw6'])
        P.act(gw[:, 6, :], gw[:, 6, :], AF.Exp, ['e2_gw6'], ['e2_gw6'])
        P.tt('dve', gw[:, 7, :], gw[:, 3, :], gw[:, 2, :], ALU.add, ['e2_gw3', 'e2_gw2'], ['e2_gw7'])
        gk = ['e2_gw%d' % i for i in range(8)]
        for hp in range(2):
            hs = [2 * hp, 2 * hp + 1]
            qb = {}
            for h in hs:
                buf = qin[h % 2]
                for i in range(3):
                    P.dma('sp', buf[:, i, :], k.dnT[i * 512 + h * 128:i * 512 + (h + 1) * 128, t0:t0 + S], [], ['e2_qin%d' % (h % 2)])
                qb[h] = buf
            for t2 in range(0, 16, 2):
                ctx = []
                for ti_ in (t2, t2 + 1):
                    for h in hs:
                        g = (h % 2) * 2 + (ti_ % 2)
                        idx = ti_ * 4 + h
                        ctx.append((h, g, idx, qb[h], 'e2_qin%d' % (h % 2), ti_, slice(ti_ * 128, (ti_ + 1) * 128)))
                for (h, g, idx, qb_, kq, ti, tsl) in ctx:
                    P.ts('dve', rhsd[g][:, 0, :], k.id32[:], gw[:, 3, idx:idx + 1], None, ALU.mult, None, ['id32', 'e2_gw3'],
                         ['e2_rhsd%d' % g])
                    P.ts('dve', rhsd[g][:, 1, :], k.id32[:], gw[:, 7, idx:idx + 1], None, ALU.mult, None, ['id32', 'e2_gw7'],
                         ['e2_rhsd%d' % g])
                for (h, g, idx, qb_, kq, ti, tsl) in ctx:
                    P.mm(X[g][:, 0:256], k.ones32[:], rhsd[g][:].rearrange("p a n -> p (a n)"), True, True,
                         ['ones32', 'e2_rhsd%d' % g], ['e2_X%da' % g])
                    P.mm(Y[g][:, 0:128], qb_[:, 1, tsl], qb_[:, 0, tsl], True, True, [kq], ['e2_Y%da' % g])
                    P.mm(Y[g][:, 128:256], qb_[:, 1, tsl], qb_[:, 1, tsl], True, True, [kq], ['e2_Y%da' % g])
                for (h, g, idx, qb_, kq, ti, tsl) in ctx:
                    P.act(t1[g][:], X[g][:, 0:256], AF.Relu, ['e2_X%da' % g, 'e2_gw3'], ['e2_t1%d' % g],
                          bias=gw[:, 3, idx:idx + 1], scale=-1.0)
                    P.act(egc[g][:], X[g][:, 0:128], AF.Exp, ['e2_X%da' % g], ['e2_egc%d' % g])
                    P.act(t1[g][:], t1[g][:], AF.Exp, ['e2_t1%d' % g], ['e2_t1%d' % g], scale=-1.0)
                for (h, g, idx, qb_, kq, ti, tsl) in ctx:
                    P.tt('pool', EE[g][:], t1[g][:], k.mU[:].rearrange("p a n -> p (a n)"), ALU.mult, ['e2_t1%d' % g, 'mU'],
                         ['e2_EE%d' % g])
                    P.tt('pool', qdT[:, h, tsl], qb_[:, 0, tsl], egc[g][:], ALU.mult, [kq, 'e2_egc%d' % g], [('e2_qdT', h)])
                    P.cp('dve', glast[:, h, 2 * ti:2 * ti + 2], egc[g][:, 63:128:64], ['e2_egc%d' % g], [('e2_glast', h)])
                for (h, g, idx, qb_, kq, ti, tsl) in ctx:
                    P.tt('dve', prod[g][:], Y[g][:, 0:256], EE[g][:], ALU.mult, ['e2_Y%da' % g, 'e2_EE%d' % g], ['e2_prod%d' % g])
                for (h, g, idx, qb_, kq, ti, tsl) in ctx:
                    P.mm(Y[g][:, 0:128], qb_[:, 1, tsl], k.idb[:], True, True, [kq, 'idb'], ['e2_Y%da' % g])
                    P.mm(Y[g][:, 128:256], qb_[:, 2, tsl], k.idb[:], True, True, [kq, 'idb'], ['e2_Y%da' % g])
                for (h, g, idx, qb_, kq, ti, tsl) in ctx:
                    P.cp('pool', qkT[:, h, ti, :], prod[g][:, 0:128], ['e2_prod%d' % g], [('e2_qkT', h)])
                    P.tt('pool', RR[g][0][:], k.id32[:], prod[g][:, 128:256], ALU.subtract, ['id32', 'e2_prod%d' % g],
                         ['e2_R%d_0' % g])
                    P.cp('pool', pp[g][0][:, 0:128], prod[g][:, 128:256], ['e2_prod%d' % g], ['e2_pp%d_0' % g])
                    P.mm(X[g][:, 256:384], pp[g][0][:, 0:128], k.idb[:], True, True, ['e2_pp%d_0' % g, 'idb'], ['e2_X%db' % g])
                for (h, g, idx, qb_, kq, ti, tsl) in ctx:
                    P.cp('act', pp[g][0][:, 128:256], X[g][:, 256:384], ['e2_X%db' % g], ['e2_pp%d_0' % g])
                    P.ts('dve', kbg[g][:], Y[g][:, 0:128], gw[:, 5, idx:idx + 1], None, ALU.mult, None,
                         ['e2_Y%da' % g, 'e2_gw5'], ['e2_kbg%d' % g])
                    P.ts('dve', kdec[:, h, ti, :], Y[g][:, 0:128], gw[:, 6, idx:idx + 1], None, ALU.mult, None,
                         ['e2_Y%da' % g, 'e2_gw6'], [('e2_kdec', h)])
                    P.ts('dve', vb[g][:], Y[g][:, 128:256], gw[:, 1, idx:idx + 1], None, ALU.mult, None,
                         ['e2_Y%da' % g, 'e2_gw1'], ['e2_vb%d' % g])
                for step in range(5):
                    a, b = step % 2, (step + 1) % 2
                    for (h, g, idx, qb_, kq, ti, tsl) in ctx:
                        kpa, kpb = 'e2_pp%d_%d' % (g, a), 'e2_pp%d_%d' % (g, b)
                        if step < 4:
                            P.mm(X[g][:, 0:128], pp[g][a][:, 128:256], pp[g][a][:, 0:128], True, True, [kpa], ['e2_X%da' % g])
                        P.mm(X[g][:, 128:256], pp[g][a][:, 0:128], pp[g][a][:, 128:256], True, True, [kpa], ['e2_X%da' % g])
                    for (h, g, idx, qb_, kq, ti, tsl) in ctx:
                        kpa, kpb = 'e2_pp%d_%d' % (g, a), 'e2_pp%d_%d' % (g, b)
                        if step < 4:
                            P.cp('act', pp[g][b][:], X[g][:, 0:256], ['e2_X%da' % g], [kpb])
                        else:
                            P.cp('act', pp[g][b][:, 128:256], X[g][:, 128:256], ['e2_X%da' % g], [kpb])
                    for (h, g, idx, qb_, kq, ti, tsl) in ctx:
                        kpb = 'e2_pp%d_%d' % (g, b)
                        P.mm(Y[g][:, 256:384], pp[g][b][:, 128:256], RR[g][a][:], True, True, [kpb, 'e2_R%d_%d' % (g, a)],
                             ['e2_Y%db' % g])
                    for (h, g, idx, qb_, kq, ti, tsl) in ctx:
                        P.tt('dve', RR[g][b][:], Y[g][:, 256:384], RR[g][a][:], ALU.add, ['e2_Y%db' % g, 'e2_R%d_%d' % (g, a)],
                             ['e2_R%d_%d' % (g, b)])
                for (h, g, idx, qb_, kq, ti, tsl) in ctx:
                    P.cp('pool', TTb[g][:], RR[g][1][:], ['e2_R%d_1' % g], ['e2_TTb%d' % g])
                for (h, g, idx, qb_, kq, ti, tsl) in ctx:
                    P.mm(X[g][:, 256:384], TTb[g][:], vb[g][:], True, True, ['e2_TTb%d' % g, 'e2_vb%d' % g], ['e2_X%db' % g])
                    P.mm(X[g][:, 384:512], kbg[g][:], TTb[g][:], True, True, ['e2_TTb%d' % g, 'e2_kbg%d' % g], ['e2_X%db' % g])
                for (h, g, idx, qb_, kq, ti, tsl) in ctx:
                    P.cp('act', uu[:, h, ti, :], X[g][:, 256:384], ['e2_X%db' % g], [('e2_u', h)])
                    P.cp('act', wT[:, h, tsl], X[g][:, 384:512], ['e2_X%db' % g], [('e2_wT', h)])
        for h in range(4):
            P.memset('pool', S32[h][:], 0.0, ['e2_S32_%d' % h])
            P.memset('pool', Sb[h][:], 0.0, ['e2_Sb%d' % h])
            P.memset('pool', vnb[h][:], 0.0, ['e2_vnb%d' % h])
        for c in range(32):
            ti, r = c // 2, c % 2
            rs = slice(r * 64, r * 64 + 64)
            tsl = slice(ti * 128, (ti + 1) * 128)
            csl = slice(c * 64, (c + 1) * 64)
            for h in range(4):
                P.mm(Y[h][:, 0:128], wT[:, h, tsl], Sb[h][:], True, True, [('e2_wT', h), 'e2_Sb%d' % h], ['e2_Y%da' % h])
            for h in range(4):
                P.tt('dve', vnb[h][rs, :], uu[rs, h, ti, :], Y[h][rs, 0:128], ALU.subtract, [('e2_u', h), 'e2_Y%da' % h],
                     ['e2_vnb%d' % h])
            for h in range(4):
                P.mm(X[h][:, 0:64], Sb[h][:], qdT[:, h, csl], True, False, ['e2_Sb%d' % h, ('e2_qdT', h)], ['e2_X%da' % h])
                P.mm(X[h][:, 0:64], vnb[h][rs, :], qkT[rs, h, ti, r * 64:(r + 1) * 64], False, True,
                     ['e2_vnb%d' % h, ('e2_qkT', h)], ['e2_X%da' % h])
                P.mm(Y[h][:, 256:384], kdec[rs, h, ti, :], vnb[h][rs, :], True, True, [('e2_kdec', h), 'e2_vnb%d' % h],
                     ['e2_Y%db' % h])
            for h in range(4):
                P.cp('act', oT[:, h, csl], X[h][:, 0:64], ['e2_X%da' % h], [('e2_oT', h)])
                P.stt(S32[h][:], S32[h][:], glast[:, h, c:c + 1], Y[h][:, 256:384], ALU.mult, ALU.add,
                      ['e2_S32_%d' % h, ('e2_glast', h), 'e2_Y%db' % h], ['e2_S32_%d' % h])
                P.cp('act', Sb[h][:], S32[h][:], ['e2_S32_%d' % h], ['e2_Sb%d' % h])
        for h in range(4):
            P.dma('sp', zt[:], k.projT[1536 + h * 128:1536 + (h + 1) * 128, t0:t0 + S], [], ['e2_zt'])
            P.act(sz[:], zt[:], AF.Silu, ['e2_zt'], ['e2_sz'])
            for b in range(4):
                c_ = slice(b * 512, (b + 1) * 512)
                P.tt('dve', sq[:], oT[:, h, c_], oT[:, h, c_], ALU.mult, [('e2_oT', h)], ['e2_sq'])
                P.mm(X[b][:], k.onesb[:], sq[:], True, True, ['e2_sq', 'onesb'], ['e2_X%da' % b, 'e2_X%db' % b])
                P.act(rt[:], X[b][:], AF.Ln, ['e2_X%da' % b, 'e2_X%db' % b], ['e2_rt'], bias=EPS, scale=1.0 / 128)
                P.act(rt[:], rt[:], AF.Exp, ['e2_rt'], ['e2_rt'], scale=-0.5)
                P.tt('dve', on[:], oT[:, h, c_], rt[:], ALU.mult, [('e2_oT', h), 'e2_rt'], ['e2_on'])
                f = fin[nfin % 2]
                kf = 'e2_fin%d' % (nfin % 2)
                nfin += 1
                P.stt(f[:], on[:], ocol[:, 0:1], sz[:, c_], ALU.mult, ALU.mult, ['e2_on', 'e2_ocol', 'e2_sz'], [kf])
                P.dma('sp', k.omixT[h * 128:(h + 1) * 128, t0 + b * 512:t0 + (b + 1) * 512], f[:], [kf], [('omixT_dn', h, s, b)])
    P.pop()


def phase_outproj(P, k, w_ap, l, xsrc, xdst, pfx):
    P.push()
    W = P.sb(pfx + "_W", [128, 8, 1024], BF16)
    load_w_bf16(P, W, w_ap, pfx + '_W')
    gt = []
    for b in range(2):
        g = P.sb(pfx + "_gt%d" % b, [128, 1024], F32)
        o = OFF['gt1']
        P.dma('sp', g[:], k.modrows[l, b:b + 1, o:o + 1024].partition_broadcast(128), [], [pfx + '_gt%d' % b])
        gt.append(g)
    NB = 4
    om = [P.sb(pfx + "_om%d" % i, [128, 8, 128], BF16) for i in range(NB)]
    xt = [P.sb(pfx + "_xt%d" % i, [128, 1024], F32) for i in range(NB)]
    tmp = [P.sb(pfx + "_tmp%d" % i, [128, 1024], F32) for i in range(NB)]
    ps = [[P.ps(pfx + "_ps%d_%d" % (i, hf), [128, 512]) for hf in range(2)] for i in range(NB)]
    for ti in range(NT):
        j = ti % NB
        b = ti // 16
        tsl = slice(ti * 128, (ti + 1) * 128)
        ko, kx, kt = pfx + '_om%d' % j, pfx + '_xt%d' % j, pfx + '_tmp%d' % j
        P.dma('sp', om[j][:], k.omixT[:, tsl].rearrange("(kc p) t -> p kc t", p=128), [], [ko])
        P.dma('actq', xt[j][:], xsrc[tsl, :], [], [kx])
        for hf in range(2):
            kp = pfx + '_ps%d_%d' % (j, hf)
            for kc in range(8):
                P.mm(ps[j][hf][:], om[j][:, kc, :], W[:, kc, hf * 512:(hf + 1) * 512], kc == 0, kc == 7, [ko, pfx + '_W'], [kp])
            P.tt('dve', tmp[j][:, hf * 512:(hf + 1) * 512], ps[j][hf][:], gt[b][:, hf * 512:(hf + 1) * 512], ALU.mult,
                 [kp, pfx + '_gt%d' % b], [(kt, hf)])
        P.tt('dve', tmp[j][:], tmp[j][:], xt[j][:], ALU.add, [(kt, 0), (kt, 1), kx], [(kt, 0), (kt, 1)])
        P.dma('actq', xdst[tsl, :], tmp[j][:], [(kt, 0), (kt, 1)], [('xdst', ti)])
    P.pop()


def phase_moe(P, k, l, xsrc, xdst, pfx):
    for s in range(2):
        P.push()
        t0 = s * S
        hT = P.sb(pfx + "_hT", [128, 8, S], BF16)
        gm = P.sb(pfx + "_gm", [128, 16, 16], F32)
        P.push()
        nm = Normer(P, k, l, 2, xsrc, True)
        rw = P.sb(pfx + "_rw", [128, 8, 16], F32)
        P.dma('sp', rw[:], k.router_w.rearrange("(kc p) e -> p kc e", p=128), [], [pfx + '_rw'])
        rb = P.sb(pfx + "_rb", [128, 16], F32)
        P.dma('sp', rb[:], k.router_b.rearrange("(o e) -> o e", o=1).partition_broadcast(128), [], [pfx + '_rb'])
        h32 = [P.sb(pfx + "_h32_%d" % i, [128, 8, 128], F32) for i in range(2)]
        plg = P.ps(pfx + "_plg", [128, 16, 16])
        for tl in range(16):
            ti = s * 16 + tl
            j = tl % 2
            hk = [(pfx + '_h32_%d' % j, kc) for kc in range(8)]
            nm.tile(ti, lambda kc, j=j: h32[j][:, kc, :], hk)
            P.cp('dve' if j == 0 else 'act', hT[:, :, tl * 128:(tl + 1) * 128], h32[j][:], hk, [(pfx + '_hT', tl)])
            for kc in range(8):
                P.mm(plg[:, tl, :], h32[j][:, kc, :], rw[:, kc, :], kc == 0, kc == 7, hk + [pfx + '_rw'], [pfx + '_plg'])
        L = P.sb(pfx + "_L", [128, 16, 16], F32)
        E_ = P.sb(pfx + "_E", [128, 16, 16], F32)
        pr = P.sb(pfx + "_pr", [128, 16, 16], F32)
        sel = P.sb(pfx + "_sel", [128, 16, 16], F32)
        ps6 = P.sb(pfx + "_ps6", [128, 64, 6], F32)
        gs = P.sb(pfx + "_gs", [128, 16, 4], F32)
        oh = P.sb(pfx + "_oh", [128, 16, 4], F32)
        msk = P.sb(pfx + "_msk", [128, 16, 16], F32)
        ing = P.sb(pfx + "_ing", [128, 16, 4], F32)
        ing2 = P.sb(pfx + "_ing2", [128, 16, 4], F32)
        oh1 = P.sb(pfx + "_oh1", [128, 16, 4], F32)
        lm = P.sb(pfx + "_lm", [128, 16, 4], F32)
        col = P.sb(pfx + "_col", [128, 8, 16], F32)
        R_ = pfx + '_r'

        def bc(ap, shape):
            return ap.to_broadcast(shape)

        def dv(fn, rd, wr):
            P.op('dve', fn, [R_ + x for x in rd], [R_ + x for x in wr])
        P.cp('dve', L[:], plg[:], [pfx + '_plg'], [R_ + 'L'])
        dv(lambda e: e.tensor_reduce(col[:, 0, :], L[:], AX.X, ALU.max), ['L'], ['c0'])
        dv(lambda e: e.tensor_tensor(L[:], L[:], bc(col[:, 0, :].unsqueeze(2), [128, 16, 16]), ALU.subtract), ['L', 'c0'], ['L'])
        P.act(E_[:], L[:], AF.Exp, [R_ + 'L'], [R_ + 'E'])
        dv(lambda e: e.tensor_reduce(col[:, 1, :], E_[:], AX.X, ALU.add), ['E'], ['c1'])
        dv(lambda e: e.reciprocal(col[:, 1, :], col[:, 1, :]), ['c1'], ['c1'])
        dv(lambda e: e.tensor_tensor(pr[:], E_[:], bc(col[:, 1, :].unsqueeze(2), [128, 16, 16]), ALU.mult), ['E', 'c1'], ['pr'])
        dv(lambda e: e.tensor_tensor(sel[:], pr[:], bc(rb[:].unsqueeze(1), [128, 16, 16]), ALU.add), ['pr'], ['sel'])
        P.readers.setdefault(pfx + '_rb', [])
        s4 = sel[:].rearrange("p t (g j) -> p (t g) j", j=4)
        dv(lambda e: e.tensor_tensor(ps6[:, :, 0:3], bc(s4[:, :, 0:1], [128, 64, 3]), s4[:, :, 1:4], ALU.add), ['sel'], ['ps6a'])
        dv(lambda e: e.tensor_tensor(ps6[:, :, 3:5], bc(s4[:, :, 1:2], [128, 64, 2]), s4[:, :, 2:4], ALU.add), ['sel'], ['ps6b'])
        dv(lambda e: e.tensor_tensor(ps6[:, :, 5:6], s4[:, :, 2:3], s4[:, :, 3:4], ALU.add), ['sel'], ['ps6c'])
        dv(lambda e: e.tensor_reduce(gs[:].rearrange("p t g -> p (t g)"), ps6[:], AX.X, ALU.max), ['ps6a', 'ps6b', 'ps6c'], ['gs'])
        dv(lambda e: e.tensor_reduce(col[:, 2, :], gs[:], AX.X, ALU.max), ['gs'], ['c2'])
        dv(lambda e: e.tensor_tensor(oh[:], gs[:], bc(col[:, 2, :].unsqueeze(2), [128, 16, 4]), ALU.is_equal), ['gs', 'c2'], ['oh'])
        dv(lambda e: e.tensor_tensor(msk[:].rearrange("p t (g j) -> p (t g) j", j=4), s4,
                                     bc(oh[:].rearrange("p t g -> p (t g)").unsqueeze(2), [128, 64, 4]), ALU.mult), ['sel', 'oh'], ['msk'])
        dv(lambda e: e.tensor_reduce(ing[:], msk[:].rearrange("p t (g j) -> p t j g", j=4), AX.X, ALU.add), ['msk'], ['ing'])
        dv(lambda e: e.tensor_reduce(col[:, 3, :], ing[:], AX.X, ALU.max), ['ing'], ['c3'])
        dv(lambda e: e.tensor_tensor(oh1[:], ing[:], bc(col[:, 3, :].unsqueeze(2), [128, 16, 4]), ALU.is_equal), ['ing', 'c3'], ['oh1'])
        dv(lambda e: e.scalar_tensor_tensor(ing2[:], oh1[:], -1.0e9, ing[:], ALU.mult, ALU.add), ['oh1', 'ing'], ['ing2'])
        dv(lambda e: e.tensor_reduce(col[:, 4, :], ing2[:], AX.X, ALU.max), ['ing2'], ['c4'])
        dv(lambda e: e.tensor_tensor(lm[:], ing2[:], bc(col[:, 4, :].unsqueeze(2), [128, 16, 4]), ALU.is_equal), ['ing2', 'c4'], ['lm'])
        dv(lambda e: e.tensor_tensor(lm[:], lm[:], oh1[:], ALU.add), ['lm', 'oh1'], ['lm'])
        gm4 = gm[:].rearrange("p t (g j) -> p t g j", j=4)
        for g in range(4):
            dv(lambda e, g=g: e.tensor_tensor(gm4[:, :, g, :], lm[:], bc(oh[:, :, g:g + 1], [128, 16, 4]), ALU.mult), ['lm', 'oh'],
               ['gm%d' % g])
        gmk = ['gm%d' % g for g in range(4)]
        dv(lambda e: e.tensor_tensor(gm[:], gm[:], pr[:], ALU.mult), gmk + ['pr'], ['gm'])
        dv(lambda e: e.tensor_reduce(col[:, 5, :], gm[:], AX.X, ALU.add), ['gm'], ['c5'])
        dv(lambda e: e.reciprocal(col[:, 5, :], col[:, 5, :]), ['c5'], ['c5'])
        dv(lambda e: e.tensor_tensor(gm[:], gm[:], bc(col[:, 5, :].unsqueeze(2), [128, 16, 16]), ALU.mult), ['gm', 'c5'], ['gm'])
        if getattr(k, 'dbg_gates', None) is not None and l == 0:
            P.dma('sp', k.dbg_gates[s], gm[:], [R_ + 'gm'], [('dbg_gates', s)])
        P.pop()
        acc = P.sb(pfx + "_acc", [128, 16, 1024], F32)
        wg = [P.sb(pfx + "_wg%d" % i, [128, 8, 256], BF16) for i in range(2)]
        wu = [P.sb(pfx + "_wu%d" % i, [128, 8, 256], BF16) for i in range(2)]
        wd = [P.sb(pfx + "_wd%d" % i, [128, 2, 1024], BF16) for i in range(2)]
        actT = [P.sb(pfx + "_actT%d" % i, [128, 2, S], BF16) for i in range(2)]
        sg = [P.sb(pfx + "_sg%d" % i, [128, 512], F32) for i in range(2)]
        psg = [P.ps(pfx + "_psg%d" % i, [128, 512]) for i in range(2)]
        psu = [P.ps(pfx + "_psu%d" % i, [128, 512]) for i in range(2)]
        psy = [P.ps(pfx + "_psy%d" % i, [128, 512]) for i in range(4)]
        hk = [(pfx + '_hT', tl) for tl in range(16)]
        st_ = {'it': 0, 'ny': 0}

        def down_unit(e_, tl, half):
            j = e_ % 2
            blk = tl // 4
            tsl = slice(tl * 128, (tl + 1) * 128)
            iy = st_['ny'] % 4
            st_['ny'] += 1
            py = psy[iy]
            ky = pfx + '_psy%d' % iy
            for c in range(2):
                P.mm(py[:], actT[j][:, c, tsl], wd[j][:, c, half * 512:(half + 1) * 512], c == 0, c == 1,
                     [(pfx + '_actT%d' % j, blk), pfx + '_wd%d' % j], [ky])
            dst = acc[:, tl, half * 512:(half + 1) * 512]
            ka = (pfx + '_acc', tl, half)
            gcol = gm[:, tl, e_:e_ + 1]
            if e_ == 0:
                P.ts('dve', dst, py[:], gcol, None, ALU.mult, None, [ky, pfx + '_rgm'], [ka])
            else:
                P.stt(dst, py[:], gcol, dst, ALU.mult, ALU.add, [ky, ka, pfx + '_rgm'], [ka])

        for e_ in range(17):
            j = e_ % 2
            pend = [(e_ - 1, tl, half) for tl in range(16) for half in range(2)] if e_ > 0 else []
            if e_ == 16:
                for u_ in pend:
                    down_unit(*u_)
                break
            for kc in range(8):
                P.dma('poolq', wg[j][:, kc, :], k.moe_w_gate[l, e_, kc * 128:(kc + 1) * 128, :], [], [pfx + '_wg%d' % j])
                P.dma('poolq', wu[j][:, kc, :], k.moe_w_up[l, e_, kc * 128:(kc + 1) * 128, :], [], [pfx + '_wu%d' % j])
            for c in range(2):
                P.dma('poolq', wd[j][:, c, :], k.moe_w_down[l, e_, c * 128:(c + 1) * 128, :], [], [pfx + '_wd%d' % j])
            for blk in range(4):
                bs = slice(blk * 512, (blk + 1) * 512)
                hkb = hk[blk * 4:(blk + 1) * 4]
                for hf in range(2):
                    i2 = st_['it'] % 2
                    st_['it'] += 1
                    kg, ku = pfx + '_psg%d' % i2, pfx + '_psu%d' % i2
                    for kc in range(8):
                        P.mm(psg[i2][:], wg[j][:, kc, hf * 128:(hf + 1) * 128], hT[:, kc, bs], kc == 0, kc == 7,
                             [pfx + '_wg%d' % j] + hkb, [kg])
                        if kc % 4 == 3 and pend:
                            down_unit(*pend.pop(0))
                    for kc in range(8):
                        P.mm(psu[i2][:], wu[j][:, kc, hf * 128:(hf + 1) * 128], hT[:, kc, bs], kc == 0, kc == 7,
                             [pfx + '_wu%d' % j] + hkb, [ku])
                        if kc % 4 == 3 and pend:
                            down_unit(*pend.pop(0))
                    P.act(sg[i2][:], psg[i2][:], AF.Silu, [kg], [pfx + '_sg%d' % i2])
                    P.tt('dve', actT[j][:, hf, bs], psu[i2][:], sg[i2][:], ALU.mult, [ku, pfx + '_sg%d' % i2],
                         [(pfx + '_actT%d' % j, blk)])
            assert not pend
        gt = P.sb(pfx + "_gt", [128, 1024], F32)
        o = OFF['gt2']
        P.dma('sp', gt[:], k.modrows[l, s:s + 1, o:o + 1024].partition_broadcast(128), [], [pfx + '_gt'])
        xt = [P.sb(pfx + "_xt%d" % i, [128, 1024], F32) for i in range(2)]
        for tl in range(16):
            j = tl % 2
            tsl = slice(t0 + tl * 128, t0 + (tl + 1) * 128)
            kx = pfx + '_xt%d' % j
            ka = [(pfx + '_acc', tl, 0), (pfx + '_acc', tl, 1)]
            P.dma('actq', xt[j][:], xsrc[tsl, :], [], [kx])
            P.tt('dve', acc[:, tl, :], acc[:, tl, :], gt[:], ALU.mult, ka + [pfx + '_gt'], ka)
            P.tt('dve', xt[j][:], xt[j][:], acc[:, tl, :], ALU.add, ka + [kx], [kx])
            P.dma('sp', xdst[tsl, :], xt[j][:], [kx], [('xdst', tl)])
        P.pop()


def phase_o1(P, k, xsrc):
    P.push()
    W = P.sb("o1_W", [128, 8, 2048], BF16)
    load_w_bf16(P, W, k.od_w_in[0], 'o1_W')
    nm = Normer(P, k, 1, 1, xsrc, False)
    hT = [P.sb("o1_hT%d" % i, [128, 8, 512], BF16) for i in range(2)]
    psf = [P.ps("o1_psf%d" % i, [128, 512]) for i in range(4)]
    psv = P.ps("o1_psv", [128, 512])
    stg = [P.sb("o1_stg%d" % i, [128, 512], BF16) for i in range(4)]
    stv = [P.sb("o1_stv%d" % i, [128, 512], BF16) for i in range(2)]
    nst = [0]

    def norm_a(tb, j):
        nm.tile_a(tb * 4 + j)

    def norm_b(tb, j):
        h = hT[tb % 2]
        hk = [('o1_hT%d' % (tb % 2), kc) for kc in range(8)]
        nm.tile_b(tb * 4 + j, lambda kc, h=h, j=j: h[:, kc, j * 128:(j + 1) * 128], hk)

    def fillers(tb):
        fs = [lambda: norm_a(tb, 0)]
        for j in range(1, 4):
            fs.append(lambda j=j: (norm_b(tb, j - 1), norm_a(tb, j)))
        fs.append(lambda: norm_b(tb, 3))
        return fs

    def units(tb):
        h = hT[tb % 2]
        hk = [('o1_hT%d' % (tb % 2), kc) for kc in range(8)]
        us = []

        def ftile(f):
            ib = f % 4
            ps = psf[ib]
            for kc in range(8):
                P.mm(ps[:], W[:, kc, f * 128:(f + 1) * 128], h[:, kc, :], kc == 0, kc == 7, ['o1_W'] + hk, ['o1_psf%d' % ib])
            sg = stg[nst[0] % 4]
            sk = 'o1_stg%d' % (nst[0] % 4)
            nst[0] += 1
            P.cp('act' if ib % 2 == 0 else 'dve', sg[:], ps[:], ['o1_psf%d' % ib], [sk])
            P.dma('sp', k.proj1T[f * 128:(f + 1) * 128, tb * 512:(tb + 1) * 512], sg[:], [sk], [('proj1T', f, tb)])

        def svtile(j):
            for kc in range(8):
                P.mm(psv[:], h[:, kc, j * 128:(j + 1) * 128], W[:, kc, 1536:2048], kc == 0, kc == 7, ['o1_W'] + hk, ['o1_psv'])
            sv = stv[j % 2]
            P.cp('act', sv[:], psv[:], ['o1_psv'], ['o1_stv%d' % (j % 2)])
            t0 = tb * 512 + j * 128
            P.dma('sp', k.svtm[t0:t0 + 128, :], sv[:], ['o1_stv%d' % (j % 2)], [('svtm', tb, j)])

        for f in range(12):
            us.append(lambda f=f: ftile(f))
        for j in range(4):
            us.append(lambda j=j: svtile(j))
        return us

    for f_ in fillers(0):
        f_()
    for tb in range(8):
        us = units(tb)
        fill = fillers(tb + 1) if tb + 1 < 8 else []
        for i, u_ in enumerate(us):
            u_()
            if fill and i % 3 == 1:
                fill.pop(0)()
        while fill:
            fill.pop(0)()
    P.pop()


def phase_o2(P, k):
    P.push()
    cw = P.sb("o2_cw", [128, 4, 4], F32)
    for kk in range(4):
        P.dma('sp', cw[:, :, kk], k.od_conv_w[0, kk, :].rearrange("(bl p) -> p bl", p=128), [], ['o2_cw'], allow_slow_non_contiguous=True)
    cols = P.sb("o2_cols", [128, 5, 4], F32)
    for i, ap in enumerate([k.od_conv_b, k.od_lru_br, k.od_lru_bi, k.od_lru_lambda]):
        P.dma('sp', cols[:, i, :], ap[0, :].rearrange("(bl p) -> p bl", p=128), [], ['o2_cols'], allow_slow_non_contiguous=True)
    P.act(cols[:, 4, :], cols[:, 3, :], AF.Exp, ['o2_cols'], ['o2_cols'], scale=-1.0)
    P.act(cols[:, 4, :], cols[:, 4, :], AF.Ln, ['o2_cols'], ['o2_cols'], bias=1.0)
    P.ts('dve', cols[:, 4, :], cols[:, 4, :], -8.0, None, ALU.mult, None, ['o2_cols'], ['o2_cols'])
    dg = P.sb("o2_dg", [128, 4, 4, 128], BF16)
    for bl in range(4):
        for kk in range(4):
            P.ts('dve', dg[:, bl, kk, :], k.id32[:], cw[:, bl, kk:kk + 1], None, ALU.mult, None, ['o2_cw', 'id32'], ['o2_dg'])
    wr = P.sb("o2_wr", [128, 4, 128], BF16)
    wi = P.sb("o2_wi", [128, 4, 128], BF16)
    for bl in range(4):
        P.dma('poolq', wr[:, bl, :], k.od_lru_wr[0, bl], [], ['o2_wr'])
        P.dma('poolq', wi[:, bl, :], k.od_lru_wi[0, bl], [], ['o2_wi'])
    raw = [P.sb("o2_raw%d" % i, [128, 3 + S], BF16) for i in range(2)]
    for i in range(2):
        P.memset('pool', raw[i][:, 0:3], 0.0, ['o2_raw%d' % i])
    lg = [P.sb("o2_lg%d" % i, [128, S], BF16) for i in range(2)]
    xc_2 = [P.sb("o2_xc_%d" % i, [128, S], F32) for i in range(2)]
    xcb_2 = [P.sb("o2_xcb_%d" % i, [128, S], BF16) for i in range(2)]
    aa_2 = [P.sb("o2_a_%d" % i, [128, S], F32) for i in range(2)]
    bb_2 = [P.sb("o2_b_%d" % i, [128, S], F32) for i in range(2)]
    ig_2 = [P.sb("o2_ig_%d" % i, [128, S], F32) for i in range(2)]
    hh_2 = [P.sb("o2_h_%d" % i, [128, S], F32) for i in range(2)]
    gl_2 = [P.sb("o2_gl_%d" % i, [128, S], F32) for i in range(2)]
    ob = [P.sb("o2_ob%d" % i, [128, S], BF16) for i in range(2)]
    psc = [P.ps("o2_psc%d" % i, [128, 512]) for i in range(2)]
    psr = [P.ps("o2_psr%d" % i, [128, 512]) for i in range(2)]
    psi = [P.ps("o2_psi%d" % i, [128, 512]) for i in range(2)]
    ri_2 = [P.sb("o2_ri_%d" % i, [128, 2, S], F32) for i in range(2)]
    n = 0
    for s in range(2):
        t0 = s * S
        for bl in range(4):
            j = n % 2
            n += 1
            kr, kl, ko = 'o2_raw%d' % j, 'o2_lg%d' % j, 'o2_ob%d' % j
            xc, xcb, aa, bb, ig, hh, gl, ri = xc_2[j], xcb_2[j], aa_2[j], bb_2[j], ig_2[j], hh_2[j], gl_2[j], ri_2[j]
            P.dma('sp', raw[j][:, 3:3 + S], k.proj1T[bl * 128:(bl + 1) * 128, t0:t0 + S], [], [kr])
            P.dma('sp', lg[j][:], k.proj1T[512 + bl * 128:512 + (bl + 1) * 128, t0:t0 + S], [], [kl])
            allb = lambda nm_: [(nm_, b) for b in range(4)]
            for b in range(4):
                c_ = slice(b * 512, (b + 1) * 512)
                pc = psc[b % 2]
                kc_ = 'o2_psc%d' % (b % 2)
                for kk in range(4):
                    P.mm(pc[:], dg[:, bl, kk, :], raw[j][:, b * 512 + kk:b * 512 + kk + 512], kk == 0, kk == 3, ['o2_dg', kr], [kc_])
                P.ts('dve', xc[:, c_], pc[:], cols[:, 0, bl:bl + 1], None, ALU.add, None, [kc_, 'o2_cols'], [('o2_xc%d' % j, b)])
                P.cp('pool', xcb[:, c_], xc[:, c_], [('o2_xc%d' % j, b)], [('o2_xcb%d' % j, b)])
                pr_, pi_ = psr[b % 2], psi[b % 2]
                P.mm(pr_[:], wr[:, bl, :], xcb[:, c_], True, True, ['o2_wr', ('o2_xcb%d' % j, b)], ['o2_psr%d' % (b % 2)])
                P.mm(pi_[:], wi[:, bl, :], xcb[:, c_], True, True, ['o2_wi', ('o2_xcb%d' % j, b)], ['o2_psi%d' % (b % 2)])
                P.ts('dve', ri[:, 0, c_], pr_[:], cols[:, 1, bl:bl + 1], None, ALU.add, None, ['o2_psr%d' % (b % 2), 'o2_cols'],
                     [('o2_r%d' % j, b)])
                P.ts('dve', ri[:, 1, c_], pi_[:], cols[:, 2, bl:bl + 1], None, ALU.add, None, ['o2_psi%d' % (b % 2), 'o2_cols'],
                     [('o2_i%d' % j, b)])
            P.act(ri[:], ri[:], AF.Sigmoid, allb('o2_r%d' % j) + allb('o2_i%d' % j), ['o2_ri%d' % j])
            P.act(aa[:], ri[:, 0, :], AF.Exp, ['o2_ri%d' % j, 'o2_cols'], ['o2_a%d' % j], scale=cols[:, 4, bl:bl + 1])
            P.tt('dve', bb[:], aa[:], aa[:], ALU.mult, ['o2_a%d' % j], ['o2_b%d' % j])
            P.act(bb[:], bb[:], AF.Sqrt, ['o2_b%d' % j], ['o2_b%d' % j], bias=1.0, scale=-1.0)
            P.tt('pool', ig[:], ri[:, 1, :], xc[:], ALU.mult, ['o2_ri%d' % j] + allb('o2_xc%d' % j), ['o2_ig%d' % j])
            P.tt('dve', bb[:], bb[:], ig[:], ALU.mult, ['o2_b%d' % j, 'o2_ig%d' % j], ['o2_b%d' % j])
            P.act(gl[:], lg[j][:], AF.Gelu_apprx_tanh, [kl], ['o2_gl%d' % j])
            P.op('dve', lambda e, hh=hh, aa=aa, bb=bb: e.tensor_tensor_scan(hh[:], aa[:], bb[:], 0.0, ALU.mult, ALU.add), ['o2_a%d' % j, 'o2_b%d' % j], ['o2_h%d' % j])
            P.tt('dve', ob[j][:], hh[:], gl[:], ALU.mult, ['o2_h%d' % j, 'o2_gl%d' % j], [ko])
            P.dma('sp', k.omixT[bl * 128:(bl + 1) * 128, t0:t0 + S], ob[j][:], [ko], [('omixT_lru', bl, s)])
    P.pop()


def phase_o3(P, k):
    P.push()
    wsT = P.sb("o3_wsT", [128, 4, 128], BF16)
    wtmp = P.sb("o3_wtmp", [128, 4, 128], F32)
    pw = P.ps("o3_pw", [128, 4, 128])
    P.dma('sp', wtmp[:], k.od_sgu_w[0].rearrange("g t s -> t g s"), [], ['o3_wtmp'])
    for g in range(4):
        P.mm(pw[:, g, :], wtmp[:, g, :], k.id32[:], True, True, ['o3_wtmp', 'id32'], ['o3_pw'])
    P.tt('dve', wsT[:], pw[:], k.mUf[:].unsqueeze(1).to_broadcast([128, 4, 128]), ALU.mult, ['o3_pw', 'mUf'], ['o3_wsT'])
    gb = P.sb("o3_gb", [128, 512], F32)
    bsb = P.sb("o3_bsb", [128, 512], F32)
    P.dma('sp', gb[:], k.od_sgu_norm_g[0:1, :].partition_broadcast(128), [], ['o3_gb'])
    P.dma('sp', bsb[:], k.od_sgu_b[0:1].rearrange("o g t -> o (g t)").partition_broadcast(128), [], ['o3_bsb'])
    G4 = 4
    sv = [P.sb("o3_sv%d" % i, [128, 512], BF16) for i in range(G4)]
    su = [P.sb("o3_su%d" % i, [128, 4, 128], BF16) for i in range(G4)]
    vg = [P.sb("o3_vg%d" % i, [128, 512], F32) for i in range(G4)]
    ug = [P.sb("o3_ug%d" % i, [128, 512], F32) for i in range(G4)]
    junk = P.sb("o3_junk", [128, 512], BF16)
    st = P.sb("o3_st", [128, 3, G4], F32)
    vn = [P.sb("o3_vn%d" % i, [128, 512], F32) for i in range(2)]
    vb = [P.sb("o3_vb%d" % i, [128, 512], BF16) for i in range(2)]
    mx = [P.sb("o3_mx%d" % i, [128, 512], F32) for i in range(2)]
    ob = [P.sb("o3_ob%d" % i, [128, 4, 128], BF16) for i in range(2)]
    pm = [P.ps("o3_pm%d" % i, [128, 4, 128]) for i in range(2)]
    for grp in range(NT // G4):
        for t in range(G4):
            ti = grp * G4 + t
            tsl = slice(ti * 128, (ti + 1) * 128)
            P.dma('sp', sv[t][:], k.svtm[tsl, :], [], ['o3_sv%d' % t])
            P.dma('sp', su[t][:], k.proj1T[1024:1536, tsl].rearrange("(g p) t -> p g t", p=128), [], ['o3_su%d' % t])
        for t in range(G4):
            P.act(vg[t][:], sv[t][:], AF.Gelu_apprx_tanh, ['o3_sv%d' % t], ['o3_vg%d' % t])
            P.act(ug[t][:], su[t][:].rearrange("p g t -> p (g t)"), AF.Gelu_apprx_tanh, ['o3_su%d' % t], ['o3_ug%d' % t])
        for t in range(G4):
            P.act(junk[:], vg[t][:], AF.Square, ['o3_vg%d' % t], ['o3_junk', 'o3_st'], accum_out=st[:, 0, t:t + 1])
        P.act(st[:, 1, :], st[:, 0, :], AF.Sqrt, ['o3_st'], ['o3_st'], bias=EPS, scale=1.0 / 512)
        P.recip(st[:, 2, :], st[:, 1, :], ['o3_st'], ['o3_st'])
        for t in range(G4):
            ti = grp * G4 + t
            j = ti % 2
            tsl = slice(ti * 128, (ti + 1) * 128)
            ko, kp = 'o3_ob%d' % j, 'o3_pm%d' % j
            P.act(vn[j][:], vg[t][:], AF.Copy, ['o3_vg%d' % t, 'o3_st'], ['o3_vn%d' % j], scale=st[:, 2, t:t + 1])
            P.tt('dve', vb[j][:], vn[j][:], gb[:], ALU.mult, ['o3_vn%d' % j, 'o3_gb'], ['o3_vb%d' % j])
            for g in range(4):
                P.mm(pm[j][:, g, :], vb[j][:, g * 128:(g + 1) * 128], wsT[:, g, :], True, True, ['o3_vb%d' % j, 'o3_wsT'], [kp])
            P.tt('dve', mx[j][:], pm[j][:].rearrange("p g t -> p (g t)"), bsb[:], ALU.add, [kp, 'o3_bsb'], ['o3_mx%d' % j])
            P.tt('dve', ob[j][:].rearrange("p g t -> p (g t)"), ug[t][:], mx[j][:], ALU.mult, ['o3_ug%d' % t, 'o3_mx%d' % j], [ko])
            P.dma('sp', k.omixT[512:1024, tsl].rearrange("(g p) t -> p g t", p=128), ob[j][:], [ko], [('omixT_sgu', ti)])
    P.pop()


def kernel(**inputs):
    n = 8
    nc, _ = build()
    shared = {}
    in_maps = []
    for core in range(n):
        m = {}
        for name, shape in IN_SPECS:
            if name == "x":
                a = np.ascontiguousarray(inputs["x"][2 * core:2 * core + 2], dtype=np.float32).reshape(T, D)
            elif name == "c":
                a = np.ascontiguousarray(inputs["c"][2 * core:2 * core + 2], dtype=np.float32)
            else:
                if name not in shared:
                    shared[name] = np.ascontiguousarray(inputs[name], dtype=np.float32).reshape(shape)
                a = shared[name]
            m[name] = a
        in_maps.append(m)
    res = run_bass_kernel_spmd(nc, in_maps, core_ids=list(range(n)))
    out = np.concatenate([np.asarray(res.results[c]["out"], dtype=np.float32).reshape(2, S, D) for c in range(n)], axis=0)
    return out
```
